# Optimizing a Trainium2 kernel written in Bass

```python
import math
import jax, jax.numpy as jnp
from jax import lax
import numpy as np

D_MODEL = 1024
BATCH = 4
SEQ = 8192
DEPTH = 1
DEC_BATCH = 4
DEC_SEQ = 4096
PAST_LEN = 128

POOL_WIDTH = 512
POOL_WINDOWS = (2, 4, 8, 16)
N_POOL_GROUPS = 4
POOL_GROUP_CH = POOL_WIDTH // N_POOL_GROUPS
N_Q_HEADS = 16
N_KV_HEADS = 4
HEAD_DIM = 64
GQA_GROUP = N_Q_HEADS // N_KV_HEADS
ATTN_WIDTH = N_Q_HEADS * HEAD_DIM
KV_WIDTH = N_KV_HEADS * HEAD_DIM
WINDOW = 128
BLOCK = 128
N_REL_BUCKETS = 32
REL_MAX_DISTANCE = 128
MEM_TOKENS = 256
MEM_HEADS = 4
MEM_HEAD_DIM = 128
MEM_WIDTH = MEM_HEADS * MEM_HEAD_DIM
N_BRANCHES = 3
IN_WIDTH = POOL_WIDTH + ATTN_WIDTH + 2 * KV_WIDTH + MEM_WIDTH + N_BRANCHES * D_MODEL
N_EXPERT_GROUPS = 4
EXPERTS_PER_GROUP = 8
N_EXPERTS = N_EXPERT_GROUPS * EXPERTS_PER_GROUP
TOP_K = 2
EXPERT_HIDDEN = 512
MOE_BLOCK = 128
ALPHA = (2 * DEPTH) ** 0.25
BETA = (8 * DEPTH) ** -0.25
LN_EPS = 1e-5
NEG_INF = -1e30

kernel_name = 'hybrid_pool_swa_mem_hmoe_encoder'


def layer_norm(x, g, b):
    xf = x.astype(jnp.float32)
    mu = jnp.mean(xf, axis=-1, keepdims=True)
    var = jnp.mean(jnp.square(xf - mu), axis=-1, keepdims=True)
    return ((xf - mu) * lax.rsqrt(var + LN_EPS) * g.astype(jnp.float32) + b.astype(jnp.float32)).astype(x.dtype)


def t5_bucket(rel):
    nb = N_REL_BUCKETS // 2
    max_exact = nb // 2
    ret = jnp.where(rel > 0, nb, 0)
    n = jnp.abs(rel)
    nf = jnp.maximum(n, 1).astype(jnp.float32)
    large = max_exact + (jnp.log(nf / max_exact) / math.log(REL_MAX_DISTANCE / max_exact) * (nb - max_exact)).astype(jnp.int32)
    large = jnp.minimum(large, nb - 1)
    return ret + jnp.where(n < max_exact, n, large)


def multiscale_pool(u, w_pool, pool_scale):
    B, S, _ = u.shape
    ug = u.astype(jnp.float32).reshape(B, S, N_POOL_GROUPS, POOL_GROUP_CH)
    cs = jnp.pad(jnp.cumsum(ug, axis=1), ((0, 0), (1, 0), (0, 0), (0, 0)))
    pos = jnp.arange(S)
    pooled = []
    for gi, win in enumerate(POOL_WINDOWS):
        lo = jnp.clip(pos - win // 2, 0, S)
        hi = jnp.clip(pos + win // 2, 0, S)
        cnt = (hi - lo).astype(jnp.float32)[None, :, None]
        mean = (cs[:, hi, gi] - cs[:, lo, gi]) / cnt
        pooled.append(mean - ug[:, :, gi])
    pooled = jnp.stack(pooled, axis=2).astype(u.dtype)
    mixed = jnp.einsum('bsgc,gcd->bsgd', pooled, w_pool) * pool_scale.reshape(N_POOL_GROUPS, POOL_GROUP_CH)
    return mixed.reshape(B, S, POOL_WIDTH)


def windowed_gqa(q, k, v, rel_table, sink):
    B, S, _ = q.shape
    nb = S // BLOCK
    qb = q.reshape(B, nb, BLOCK, N_KV_HEADS, GQA_GROUP, HEAD_DIM)
    pad = ((0, 0), (BLOCK, BLOCK), (0, 0))
    kp = jnp.pad(k, pad).reshape(B, nb + 2, BLOCK, N_KV_HEADS, HEAD_DIM)
    vp = jnp.pad(v, pad).reshape(B, nb + 2, BLOCK, N_KV_HEADS, HEAD_DIM)
    kb = jnp.concatenate([kp[:, :-2], kp[:, 1:-1], kp[:, 2:]], axis=2)
    vb = jnp.concatenate([vp[:, :-2], vp[:, 1:-1], vp[:, 2:]], axis=2)
    t = jnp.arange(BLOCK)[:, None]
    j = jnp.arange(3 * BLOCK)[None, :]
    rel = j - BLOCK - t
    bias = rel_table[t5_bucket(rel)].astype(jnp.float32)
    bias = jnp.transpose(bias, (2, 0, 1)).reshape(N_KV_HEADS, GQA_GROUP, BLOCK, 3 * BLOCK)
    kpos = jnp.arange(nb)[:, None, None] * BLOCK - BLOCK + j[None]
    valid = (jnp.abs(rel) <= WINDOW)[None] & (kpos >= 0) & (kpos < S)
    s = jnp.einsum('bnqhgd,bnkhd->bnhgqk', qb, kb, preferred_element_type=jnp.float32) * (HEAD_DIM ** -0.5)
    s = jnp.where(valid[None, :, None, None], s + bias[None, None], NEG_INF)
    sink_b = sink.astype(jnp.float32).reshape(N_KV_HEADS, GQA_GROUP)[None, None, :, :, None, None]
    m = jnp.maximum(jnp.max(s, axis=-1, keepdims=True), sink_b)
    p = jnp.exp(s - m)
    denom = jnp.sum(p, axis=-1, keepdims=True) + jnp.exp(sink_b - m)
    o = jnp.einsum('bnhgqk,bnkhd->bnqhgd', (p / denom).astype(v.dtype), vb)
    return o.reshape(B, S, ATTN_WIDTH)


def memory_attention(qm, mem, w_mem_kv):
    B, S, _ = qm.shape
    kv = mem @ w_mem_kv
    km = kv[..., :MEM_WIDTH].reshape(B, -1, MEM_HEADS, MEM_HEAD_DIM)
    vm = kv[..., MEM_WIDTH:].reshape(B, -1, MEM_HEADS, MEM_HEAD_DIM)
    q4 = qm.reshape(B, S, MEM_HEADS, MEM_HEAD_DIM)
    s = jnp.einsum('bshd,bmhd->bhsm', q4, km, preferred_element_type=jnp.float32) * (MEM_HEAD_DIM ** -0.5)
    p = jax.nn.softmax(s, axis=-1).astype(vm.dtype)
    o = jnp.einsum('bhsm,bmhd->bshd', p, vm)
    return o.reshape(B, S, MEM_WIDTH)


def hier_moe(x, w_router_group, w_router_expert, w_gu, w_down):
    B, S, D = x.shape
    T = B * S
    A = T * TOP_K
    xt = x.reshape(T, D)
    ar = jnp.arange(T)
    gl = (xt @ w_router_group).astype(jnp.float32)
    gp = jax.nn.softmax(gl, axis=-1)
    grp = jnp.argmax(gl, axis=-1)
    el = (xt @ w_router_expert).astype(jnp.float32).reshape(T, N_EXPERT_GROUPS, EXPERTS_PER_GROUP)
    el_sel = el[ar, grp]
    top_l, top_i = lax.top_k(el_sel, TOP_K)
    wts = jax.nn.softmax(top_l, axis=-1) * gp[ar, grp][:, None]
    eid = (grp[:, None] * EXPERTS_PER_GROUP + top_i).reshape(A)
    tok = jnp.repeat(ar, TOP_K)
    wflat = wts.reshape(A)
    order = jnp.argsort(eid, stable=True)
    se, stok, sw = eid[order], tok[order], wflat[order]
    counts = jax.ops.segment_sum(jnp.ones((A,), jnp.int32), eid, num_segments=N_EXPERTS)
    starts = jnp.cumsum(counts) - counts
    padc = (counts + MOE_BLOCK - 1) // MOE_BLOCK * MOE_BLOCK
    pends = jnp.cumsum(padc)
    pstarts = pends - padc
    dest = pstarts[se] + jnp.arange(A) - starts[se]
    P = A + N_EXPERTS * MOE_BLOCK
    nblk = P // MOE_BLOCK
    buf_tok = jnp.full((P,), T, jnp.int32).at[dest].set(stok)
    buf_w = jnp.zeros((P,), jnp.float32).at[dest].set(sw)
    xpad = jnp.concatenate([xt, jnp.zeros((1, D), xt.dtype)], axis=0)
    buf_x = xpad[buf_tok].reshape(nblk, MOE_BLOCK, D)
    blk_e = jnp.minimum(jnp.searchsorted(pends, jnp.arange(nblk) * MOE_BLOCK, side='right'), N_EXPERTS - 1)

    def expert_block(args):
        xb, e = args
        gu = xb @ w_gu[e]
        h = jax.nn.silu(gu[:, :EXPERT_HIDDEN]) * gu[:, EXPERT_HIDDEN:]
        return h @ w_down[e]

    yb = lax.map(expert_block, (buf_x, blk_e)).reshape(P, D)
    out = jnp.zeros((T + 1, D), yb.dtype).at[buf_tok].add(yb * buf_w[:, None].astype(yb.dtype))
    return out[:T].reshape(B, S, D)


def encoder_trunk(x, mem, ln_in_g, ln_in_b, rel_bias_table, w_in, b_in, w_pool, pool_scale, p_pool,
                  sink, p_attn, w_mem_kv, p_mem, w_out, ln1_g, ln1_b, w_router_group, w_router_expert,
                  w_gu, w_down, ln2_g, ln2_b):
    B, S, D = x.shape
    x = layer_norm(x, ln_in_g, ln_in_b)
    o1 = POOL_WIDTH
    o2 = o1 + ATTN_WIDTH
    o3 = o2 + KV_WIDTH
    o4 = o3 + KV_WIDTH
    o5 = o4 + MEM_WIDTH
    for l in range(DEPTH):
        h = x @ w_in[l] + b_in[l]
        a = multiscale_pool(h[..., :o1], w_pool[l], pool_scale[l]) @ p_pool[l]
        b = windowed_gqa(h[..., o1:o2], h[..., o2:o3], h[..., o3:o4], rel_bias_table, sink[l]) @ p_attn[l]
        c = memory_attention(h[..., o4:o5], mem, w_mem_kv[l]) @ p_mem[l]
        g = jax.nn.sigmoid(h[..., o5:]).reshape(B, S, N_BRANCHES, D)
        merged = g[:, :, 0] * a + g[:, :, 1] * b + g[:, :, 2] * c
        x = layer_norm(ALPHA * x + merged @ w_out[l], ln1_g[l], ln1_b[l])
        x = layer_norm(ALPHA * x + hier_moe(x, w_router_group[l], w_router_expert[l], w_gu[l], w_down[l]), ln2_g[l], ln2_b[l])
    return x


def setup_inputs(seed: int = 0) -> dict:
    key = jax.random.key(seed)
    ks = jax.random.split(key, 32)
    f32 = jnp.float32
    nrm = lambda k, shape, scale: jax.random.normal(k, shape, f32) * scale
    L = DEPTH
    return {
        'x_prompt': nrm(ks[0], (BATCH, SEQ, D_MODEL), 1.0),
        'x_sample': nrm(ks[1], (DEC_BATCH, DEC_SEQ, D_MODEL), 1.0),
        'mem_prompt': nrm(ks[2], (BATCH, MEM_TOKENS, D_MODEL), 1.0),
        'mem_sample': nrm(ks[3], (DEC_BATCH, MEM_TOKENS, D_MODEL), 1.0),
        'ln_in_g': 1.0 + nrm(ks[4], (D_MODEL,), 0.02),
        'ln_in_b': nrm(ks[5], (D_MODEL,), 0.02),
        'rel_bias_table': nrm(ks[6], (N_REL_BUCKETS, N_Q_HEADS), 0.1),
        'w_in': nrm(ks[7], (L, D_MODEL, IN_WIDTH), D_MODEL ** -0.5),
        'b_in': nrm(ks[8], (L, IN_WIDTH), 0.02),
        'w_pool': nrm(ks[9], (L, N_POOL_GROUPS, POOL_GROUP_CH, POOL_GROUP_CH), POOL_GROUP_CH ** -0.5),
        'pool_scale': 1.0 + nrm(ks[10], (L, POOL_WIDTH), 0.02),
        'p_pool': nrm(ks[11], (L, POOL_WIDTH, D_MODEL), POOL_WIDTH ** -0.5),
        'sink': nrm(ks[12], (L, N_Q_HEADS), 0.5),
        'p_attn': nrm(ks[13], (L, ATTN_WIDTH, D_MODEL), ATTN_WIDTH ** -0.5),
        'w_mem_kv': nrm(ks[14], (L, D_MODEL, 2 * MEM_WIDTH), D_MODEL ** -0.5),
        'p_mem': nrm(ks[15], (L, MEM_WIDTH, D_MODEL), MEM_WIDTH ** -0.5),
        'w_out': nrm(ks[16], (L, D_MODEL, D_MODEL), BETA * D_MODEL ** -0.5),
        'ln1_g': 1.0 + nrm(ks[17], (L, D_MODEL), 0.02),
        'ln1_b': nrm(ks[18], (L, D_MODEL), 0.02),
        'w_router_group': nrm(ks[19], (L, D_MODEL, N_EXPERT_GROUPS), D_MODEL ** -0.5),
        'w_router_expert': nrm(ks[20], (L, D_MODEL, N_EXPERTS), D_MODEL ** -0.5),
        'w_gu': nrm(ks[21], (L, N_EXPERTS, D_MODEL, 2 * EXPERT_HIDDEN), D_MODEL ** -0.5),
        'w_down': nrm(ks[22], (L, N_EXPERTS, EXPERT_HIDDEN, D_MODEL), BETA * EXPERT_HIDDEN ** -0.5),
        'ln2_g': 1.0 + nrm(ks[23], (L, D_MODEL), 0.02),
        'ln2_b': nrm(ks[24], (L, D_MODEL), 0.02),
    }


def reference(x_prompt, x_sample, mem_prompt, mem_sample, ln_in_g, ln_in_b, rel_bias_table, w_in, b_in,
              w_pool, pool_scale, p_pool, sink, p_attn, w_mem_kv, p_mem, w_out, ln1_g, ln1_b,
              w_router_group, w_router_expert, w_gu, w_down, ln2_g, ln2_b):
    y_prompt = encoder_trunk(x_prompt, mem_prompt, ln_in_g, ln_in_b, rel_bias_table, w_in, b_in, w_pool,
                             pool_scale, p_pool, sink, p_attn, w_mem_kv, p_mem, w_out, ln1_g, ln1_b,
                             w_router_group, w_router_expert, w_gu, w_down, ln2_g, ln2_b)
    y_sample = encoder_trunk(x_sample, mem_sample, ln_in_g, ln_in_b, rel_bias_table, w_in, b_in, w_pool,
                             pool_scale, p_pool, sink, p_attn, w_mem_kv, p_mem, w_out, ln1_g, ln1_b,
                             w_router_group, w_router_expert, w_gu, w_down, ln2_g, ln2_b)
    return (y_prompt, y_sample)
```

```python
import numpy as np
import math
import concourse.bass as bass
import concourse.mybir as mybir
from concourse.bass_utils import run_bass_kernel_spmd

F32 = mybir.dt.float32
BF16 = mybir.dt.bfloat16
I32 = mybir.dt.int32
U32 = mybir.dt.uint32
AF = mybir.ActivationFunctionType
ALU = mybir.AluOpType
AX = mybir.AxisListType

D = 1024
NCORES = 8
P_TOK = 4096
S_TOK = 2048
TOK = P_TOK + S_TOK
NBLK = TOK // 128
NT = 512
CAP = 512
NE = 32
ALPHA = 2.0 ** 0.25
LN_EPS = 1e-5
NEG = -30000.0
SEGS = [(0, P_TOK), (P_TOK + 256, S_TOK)]
XH_ROWS = P_TOK + 256 + S_TOK + 256
C_U, C_Q, C_K, C_V, C_QM, C_RES = 0, 512, 1536, 1792, 2048, 2560

DSIZE = {F32: 4, BF16: 2, I32: 4, U32: 4}


class Buf:
    def __init__(self, name, arena, lo, shape, dtype, ap):
        self.name, self.arena, self.lo, self.shape, self.dtype, self.ap = name, arena, lo, tuple(shape), dtype, ap
        n = 1
        for s in shape:
            n *= s
        self.nbytes = n * DSIZE[dtype]
        self.hi = lo + self.nbytes
        self.stride0 = self.nbytes // shape[0]

    def a(self, i0=None, i1=None):
        if i0 is None:
            return (self.arena, self.lo, self.hi)
        if i1 is None:
            i1 = i0 + 1
        return (self.arena, self.lo + i0 * self.stride0, self.lo + i1 * self.stride0)


class Op:
    __slots__ = ("idx", "eng", "fn", "dma", "deps", "signal", "sem", "val", "prev_wait")


class Sched:
    COMPUTE = ("pe", "act", "dve", "pool")

    def __init__(self, nc):
        self.nc = nc
        self.ops = []
        self.iv = {}
        self.eng_obj = {"pe": nc.tensor, "act": nc.scalar, "dve": nc.vector, "pool": nc.gpsimd, "sp": nc.sync}

    def op(self, eng, fn, r=(), w=(), dma=False, mode="full"):
        o = Op()
        o.idx = len(self.ops)
        o.eng, o.fn, o.dma = eng, fn, dma
        o.signal = False
        o.deps = []
        if mode == "none":
            self.ops.append(o)
            return o
        deps = set()
        if mode == "full":
            for (ar, lo, hi) in r:
                for e in self.iv.setdefault(ar, []):
                    if e[0] < hi and lo < e[1] and e[2] is not None:
                        deps.add(e[2])
            for (ar, lo, hi) in w:
                for e in self.iv.setdefault(ar, []):
                    if e[0] < hi and lo < e[1]:
                        if e[2] is not None:
                            deps.add(e[2])
                        deps.update(e[3].values())
                        deps.update(e[4])
        deps.discard(o.idx)
        for (ar, lo, hi) in list(r) + list(w):
            lst = self.iv.setdefault(ar, [])
            new = []
            for e in lst:
                if e[0] < hi and lo < e[1] and not (lo <= e[0] and e[1] <= hi):
                    pts = sorted(set([e[0], e[1]] + [p for p in (lo, hi) if e[0] < p < e[1]]))
                    for a_, b_ in zip(pts[:-1], pts[1:]):
                        new.append([a_, b_, e[2], dict(e[3]), list(e[4])])
                else:
                    new.append(e)
            self.iv[ar] = new
        for (ar, lo, hi) in r:
            lst = self.iv[ar]
            inside = sorted([e for e in lst if lo <= e[0] and e[1] <= hi], key=lambda e: e[0])
            cur = lo
            gaps = []
            for e in inside:
                if e[0] > cur:
                    gaps.append([cur, e[0], None, {}, []])
                cur = max(cur, e[1])
                if dma:
                    e[4].append(o.idx)
                else:
                    e[3][eng] = o.idx
            if cur < hi:
                gaps.append([cur, hi, None, {}, []])
            for g_ in gaps:
                if dma:
                    g_[4].append(o.idx)
                else:
                    g_[3][eng] = o.idx
                lst.append(g_)
        for (ar, lo, hi) in w:
            lst = self.iv[ar]
            lst[:] = [e for e in lst if not (lo <= e[0] and e[1] <= hi)]
            lst.append([lo, hi, o.idx, {}, []])
        red = {}
        out = []
        for d in deps:
            p = self.ops[d]
            if p.dma:
                out.append(d)
            else:
                if p.eng == "pe" and eng == "pe" and not dma:
                    continue
                if p.eng not in red or red[p.eng] < d:
                    red[p.eng] = d
        out.extend(red.values())
        o.deps = out
        for d in out:
            self.ops[d].signal = True
        self.ops.append(o)
        return o

    def emit(self, sems_compute, sems_dma):
        cnt = {e: 0 for e in self.COMPUTE}
        dcnt = {q: 0 for q in sems_dma}
        for o in self.ops:
            if o.dma:
                q = o.eng
                n = dcnt[q]
                ns = len(sems_dma[q])
                o.sem = sems_dma[q][n % ns]
                o.val = 16 * (n // ns + 1)
                o.prev_wait = 16 * (n // ns)
                dcnt[q] = n + 1
            else:
                o.prev_wait = 0
                if o.signal:
                    cnt[o.eng] += 1
                    o.sem = sems_compute[o.eng]
                    o.val = cnt[o.eng]
        known = {}
        nwait = 0
        for o in self.ops:
            eo = self.eng_obj[o.eng]
            kn = known.setdefault(o.eng, {})
            waits = []
            for d in o.deps:
                p = self.ops[d]
                waits.append((p.sem, p.val))
            if o.dma and o.prev_wait > 0:
                waits.append((o.sem, o.prev_wait))
            for (sem, val) in waits:
                key = id(sem)
                if kn.get(key, 0) >= val:
                    continue
                kn[key] = val
                eo.wait_ge(sem, val)
                nwait += 1
            inst = o.fn()
            if o.dma:
                inst.then_inc(o.sem, 16)
            elif o.signal:
                inst.then_inc(o.sem, 1)
        for q, n in dcnt.items():
            eo = self.eng_obj[q]
            ns = len(sems_dma[q])
            for i in range(min(n, ns)):
                last_n = n - 1 - ((n - 1 - i) % ns)
                eo.wait_ge(sems_dma[q][i], 16 * (last_n // ns + 1))
        return nwait


def build_program(debug=None):
    nc = bass.Bass("TRN2", target_bir_lowering=False)
    S = Sched(nc)

    def din(name, shape, dt=F32):
        return nc.dram_tensor(name, list(shape), dt, kind="ExternalInput").ap()

    xh = din("xh", [XH_ROWS, D])
    mem = din("mem", [2, 256, D])
    flags = din("flags", [128, 4])
    invcnt = din("invcnt", [128, 2, 2, 4, 16])
    w_res_d = din("w_res", [128, 8, C_RES])
    b_res_d = din("b_res", [128, 20])
    bv_d = din("bv_bc", [128, 256])
    b_gate_d = din("b_gate", [128, 24])
    wst_d = din("wst", [8, 128, 5120])
    w_out_d = din("w_out_l", [128, 8, D])
    w_pool_d = din("w_pool_l", [128, 4, 128])
    pscale_d = din("pscale", [128, 4])
    w_memkv_d = din("w_memkv_l", [128, 8, D])
    ln_d = din("ln_bc", [6, 128, D])
    biasT_d = din("biasT", [128, 3, 16, 128])
    sink_d = din("sink_bc", [128, 16])
    wr_d = din("wr_l", [128, 8, 36])
    w_gu_d = din("w_gu", [NE, D, D])
    w_down_d = din("w_down", [NE, 512, D])
    consts_d = din("consts", [128, 419])
    y_out = nc.dram_tensor("y", [TOK, D], F32, kind="ExternalOutput").ap()

    x1_scr = nc.dram_tensor("x1_scr", [TOK, D], F32).ap()
    xs_scr = nc.dram_tensor("xs_scr", [NE * CAP + 128, D], BF16).ap()
    ys_scr = nc.dram_tensor("ys_scr", [NE * CAP + 128, D], F32).ap()
    wst_scr = nc.dram_tensor("wst_scr", [8, 128, 5120], BF16).ap()
    wres_scr = nc.dram_tensor("wres_scr", [128, 20480], BF16).ap()

    AW = 53100
    arena = nc.alloc_sbuf_tensor("arena", [128, AW], F32)

    class Alloc:
        def __init__(self):
            self.off = 0

        def buf(self, name, shape, dt):
            n = 1
            for s in shape:
                n *= s
            nb = n * DSIZE[dt]
            nb4 = (nb + 31) // 32 * 32
            lo = self.off
            assert lo + nb4 <= AW * 4, (name, lo, nb4)
            self.off += nb4
            ap = arena[:, lo // 4:(lo + nb4) // 4]
            if dt != F32:
                ap = ap.bitcast(dt)
            ap = ap[:, 0:n]
            if len(shape) == 2:
                ap = ap.rearrange("p (a b) -> p a b", b=shape[1])
            elif len(shape) == 3:
                ap = ap.rearrange("p (a b c) -> p a b c", b=shape[1], c=shape[2])
            return Buf(name, "arena", lo, shape, dt, ap)

    A = Alloc()

    psum_t = [nc.alloc_psum_tensor(f"ps{i}", [128, 512], F32) for i in range(8)]
    pstate = {"i": 0}

    class PS:
        def __init__(self, i):
            self.i = i
            self.f = psum_t[i][:, :]
            self.b = psum_t[i][:, :].bitcast(BF16)
            self.acc = (f"psum{i}", 0, 2048)

    def psum():
        i = pstate["i"]
        pstate["i"] = (i + 1) % 8
        return PS(i)

    cst = A.buf("cst", [419], F32)
    ident_f = cst.ap[:, 0:128]
    triU = cst.ap[:, 128:256]
    ones_f = cst.ap[:, 256:384]
    ebase = cst.ap[:, 384:416]
    c_mhalf = cst.ap[:, 416:417]
    trashcol = cst.ap[:, 418:419]
    c_eps = cst.ap[:, 417:418]
    ident_b = A.buf("ident_b", [128], BF16)
    flg = A.buf("flg", [4], F32)
    icn = A.buf("icn", [2 * 2 * 4, 16], F32)
    b_res = A.buf("b_res", [20], F32)
    b_gate = A.buf("b_gate", [24], F32)
    bv = A.buf("bv", [256], F32)
    pscale = A.buf("pscale", [4], F32)
    esink = A.buf("esink", [16], F32)
    wr = A.buf("wr", [8, 36], F32)
    carry = A.buf("carry", [32], F32)
    bq8 = A.buf("bq8", [8], F32)
    lg_all = A.buf("lg_all", [4, 36], F32)
    idx_all = A.buf("idx_all", [NBLK, 2], I32)
    wts_all = A.buf("wts_all", [NBLK, 2], F32)
    kmT = [A.buf(f"kmT{s}", [4, 256], BF16) for s in range(2)]
    vmaug = [A.buf(f"vmaug{s}", [2, 4, 129], BF16) for s in range(2)]
    rsmall = A.buf("rsmall", [32, 36], F32)
    mark_phase = A.off

    ln_g_in = A.buf("ln_g_in", [D], F32)
    ln_b_in = A.buf("ln_b_in", [D], F32)
    ln_g1 = A.buf("ln_g1", [D], F32)
    ln_b1 = A.buf("ln_b1", [D], F32)
    biasT = A.buf("biasT", [3, 16, 128], BF16)
    w_out = A.buf("w_out", [8, D], BF16)
    w_pool = A.buf("w_pool", [4, 128], BF16)
    wstb = [A.buf(f"wst{i}", [40, 128], BF16) for i in range(2)]
    mark_act = A.off

    def ld(eng, dst_ap, src_ap, w, r=()):
        return S.op(eng, lambda: S.eng_obj[eng].dma_start(out=dst_ap, in_=src_ap), r=r, w=w, dma=True)

    ld("sp", cst.ap, consts_d[:, :], [cst.a()])
    ld("sp", flg.ap, flags[:, :], [flg.a()])
    ld("sp", icn.ap, invcnt.rearrange("p s e g t -> p (s e g) t"), [icn.a()])
    ld("sp", b_res.ap, b_res_d[:, :], [b_res.a()])
    ld("sp", b_gate.ap, b_gate_d[:, :], [b_gate.a()])
    ld("sp", bv.ap, bv_d[:, :], [bv.a()])
    ld("sp", pscale.ap, pscale_d[:, :], [pscale.a()])
    ld("sp", esink.ap, sink_d[:, :], [esink.a()])
    ld("sp", wr.ap, wr_d[:, :, :], [wr.a()])
    ld("sp", ln_g_in.ap, ln_d[0], [ln_g_in.a()])
    ld("sp", ln_b_in.ap, ln_d[1], [ln_b_in.a()])
    ld("sp", ln_g1.ap, ln_d[2], [ln_g1.a()])
    ld("sp", ln_b1.ap, ln_d[3], [ln_b1.a()])
    for r_ in range(3):
        ld("pool", biasT.ap[:, r_], biasT_d[:, r_], [biasT.a(r_)])
    ld("pool", w_pool.ap, w_pool_d[:, :, :], [w_pool.a()])
    S.op("act", lambda: nc.scalar.activation(out=esink.ap, in_=esink.ap, func=AF.Exp), r=[esink.a()], w=[esink.a()])
    S.op("act", lambda: nc.scalar.copy(out=ident_b.ap, in_=ident_f), r=[cst.a()], w=[ident_b.a()])
    S.op("pool", lambda: nc.gpsimd.memset(carry.ap, 0.0), w=[carry.a()])
    S.op("dve", lambda: nc.vector.tensor_scalar(out=bq8.ap, in0=b_res.ap[:, 4:12], scalar1=0.125, scalar2=None, op0=ALU.mult), r=[b_res.a()], w=[bq8.a()])
    S.op("pool", lambda: nc.gpsimd.affine_select(out=biasT.ap[:, 0], in_=biasT.ap[:, 0], pattern=[[0, 16], [-1, 128]],
                                                 compare_op=ALU.is_ge, fill=NEG, base=0, channel_multiplier=1),
         r=[biasT.a(0)], w=[biasT.a(0)])
    S.op("pool", lambda: nc.gpsimd.affine_select(out=biasT.ap[:, 2], in_=biasT.ap[:, 2], pattern=[[0, 16], [1, 128]],
                                                 compare_op=ALU.is_ge, fill=NEG, base=0, channel_multiplier=-1),
         r=[biasT.a(2)], w=[biasT.a(2)])

    def mm_group(ps_ap, pairs, ps_acc, reads):
        n = len(pairs)
        last = None
        for i, (l, r_) in enumerate(pairs):
            def f(l=l, r_=r_, i=i):
                return nc.tensor.matmul(ps_ap, l, r_, start=(i == 0), stop=(i == n - 1))
            if i == 0:
                mode = "full"
            elif i == n - 1:
                mode = "reg"
            else:
                mode = "none"
            last = S.op("pe", f, r=reads, w=[ps_acc], mode=mode)
        return last

    def ln_stats(src_ap, src_acc, stat):
        st6 = stat.ap[:, 0:12].rearrange("p (a b) -> p a b", b=6)
        for h in range(2):
            S.op("dve", lambda h=h: nc.vector.bn_stats(out=st6[:, h, :], in_=src_ap[:, h * 512:(h + 1) * 512]),
                 r=[src_acc], w=[stat.a(0)])
        S.op("dve", lambda: nc.vector.bn_aggr(out=stat.ap[:, 12:14], in_=stat.ap[:, 0:12]), r=[stat.a(0)], w=[stat.a(0)])
        S.op("act", lambda: nc.scalar.activation(out=stat.ap[:, 14:15], in_=stat.ap[:, 13:14], func=AF.Ln, bias=c_eps),
             r=[stat.a(0), cst.a()], w=[stat.a(0)])
        S.op("act", lambda: nc.scalar.activation(out=stat.ap[:, 15:16], in_=stat.ap[:, 14:15], func=AF.Exp, scale=-0.5),
             r=[stat.a(0)], w=[stat.a(0)])

    def ln_apply(src_ap, src_acc, dst_ap, dst_acc, g_buf, b_buf, stat):
        S.op("dve", lambda: nc.vector.scalar_tensor_tensor(out=dst_ap, in0=src_ap, scalar=stat.ap[:, 12:13], in1=g_buf.ap, op0=ALU.subtract, op1=ALU.mult),
             r=[src_acc, stat.a(0), g_buf.a()], w=[dst_acc])
        S.op("dve", lambda: nc.vector.scalar_tensor_tensor(out=dst_ap, in0=dst_ap, scalar=stat.ap[:, 15:16], in1=b_buf.ap, op0=ALU.mult, op1=ALU.add),
             r=[dst_acc, stat.a(0), b_buf.a()], w=[dst_acc])

    def ln_apply2(src_ap, src_acc, mid_ap, mid_acc, dst_ap, dst_acc, g_buf, b_buf, stat):
        S.op("dve", lambda: nc.vector.scalar_tensor_tensor(out=mid_ap, in0=src_ap, scalar=stat.ap[:, 12:13], in1=g_buf.ap, op0=ALU.subtract, op1=ALU.mult),
             r=[src_acc, stat.a(0), g_buf.a()], w=[mid_acc])
        S.op("dve", lambda: nc.vector.scalar_tensor_tensor(out=dst_ap, in0=mid_ap, scalar=stat.ap[:, 15:16], in1=b_buf.ap, op0=ALU.mult, op1=ALU.add),
             r=[mid_acc, stat.a(0), b_buf.a()], w=[dst_acc])

    def layer_norm_tok(src_ap, src_acc, dst_ap, dst_acc, g_buf, b_buf, tmp, stat):
        ln_stats(src_ap, src_acc, stat)
        ln_apply(src_ap, src_acc, dst_ap, dst_acc, g_buf, b_buf, stat)

    A.off = mark_act
    xin = [A.buf(f"xin{i}", [D], F32) for i in range(2)]
    lntmp = None
    lnstat = [A.buf(f"lnstat{i}", [16], F32) for i in range(2)]
    xn_c = A.buf("xn_c", [4, D], F32)
    xnb = [A.buf(f"xnb{i}", [D], BF16) for i in range(2)]
    xnT = A.buf("xnT", [8, 768], BF16)
    mixedT = A.buf("mixedT", [4, 512], BF16)
    oT = A.buf("oT", [8, 512], BF16)
    omT = A.buf("omT", [4, 512], BF16)
    xin.append(Buf("xin2", "arena", mixedT.lo, [D], F32, arena[:, mixedT.lo // 4:mixedT.lo // 4 + D]))
    xin.append(Buf("xin3", "arena", omT.lo, [D], F32, arena[:, omT.lo // 4:omT.lo // 4 + D]))
    mark_stage = A.off
    w_u = A.buf("w_u", [8, 512], BF16)
    w_q = A.buf("w_q", [8, 1024], BF16)
    w_k = A.buf("w_k", [8, 256], BF16)
    w_v = A.buf("w_v", [8, 256], BF16)
    w_qm = A.buf("w_qm", [8, 512], BF16)
    mark_bcd = A.off
    assert mark_bcd - mark_stage == 40960
    wres_flat = Buf("wres_flat", "arena", mark_stage, [20480], BF16, arena[:, mark_stage // 4:mark_bcd // 4].bitcast(BF16))
    uT = A.buf("uT", [4, 544], F32)
    ptA = A.buf("ptA", [544], F32)
    ptB = A.buf("ptB", [544], F32)
    pooledT = A.buf("pooledT", [4, 512], BF16)
    endB = A.off
    A.off = mark_bcd
    kT = A.buf("kT", [2, 768], BF16)
    vaug = A.buf("vaug", [6, 4, 65], BF16)
    PT = [A.buf(f"PT{i}", [512], BF16) for i in range(6)]
    o_sb = [A.buf(f"o_sb{i}", [D], BF16) for i in range(2)]
    den = [A.buf(f"den{i}", [8], F32) for i in range(2)]
    assert A.off <= endB
    A.off = mark_bcd + 8704 + 9600
    qT = A.buf("qT", [8, 512], BF16)
    endC = A.off
    A.off = mark_bcd
    qmT = A.buf("qmT", [4, 512], BF16)
    PmT = A.buf("PmT", [8, 512], BF16)
    om = [A.buf(f"om{i}", [512], BF16) for i in range(2)]
    dend = [A.buf(f"dend{i}", [4], F32) for i in range(2)]
    endD = A.off
    A.off = mark_stage
    mergedT = A.buf("mergedT", [8, 512], BF16)
    gsig = [A.buf(f"gsig{i}", [512], F32) for i in range(3)]
    macc = [A.buf(f"macc{i}", [512], F32) for i in range(2)]
    x1 = [A.buf(f"x1_{i}", [D], F32) for i in range(4)]
    x1T = [A.buf(f"x1T{i}", [8, 128], F32) for i in range(2)]
    lnstatF = [A.buf(f"lnstatF{i}", [16], F32) for i in range(4)]
    assert A.off <= mark_bcd + 8704
    x1b = [Buf(f"x1b{i}", "arena", oT.lo + i * 2048, [D], BF16, arena[:, (oT.lo + i * 2048) // 4:(oT.lo + (i + 1) * 2048) // 4].bitcast(BF16)) for i in range(4)]
    A.off = mark_bcd + 8704
    rbig = A.buf("rbig", [2400], F32)
    endE = A.off
    print("[kernel] sbuf marks", mark_phase, mark_act, mark_stage, endB, endC, endD, endE, AW * 4)
    assert max(endB, endC, endD, endE) <= AW * 4
    S.op("pool", lambda: nc.gpsimd.memset(xin[0].ap, 0.0), w=[xin[0].a()])
    S.op("sp", lambda: nc.sync.dma_start(out=ys_scr[NE * CAP:NE * CAP + 128, :], in_=xin[0].ap), r=[xin[0].a()], w=[("ys_scr", NE, NE + 1)], dma=True)
    col = 0
    for wb_, ncol in ((w_u, 512), (w_q, 1024), (w_k, 256), (w_v, 256), (w_qm, 512)):
        ld("pool", wb_.ap, w_res_d[:, :, col:col + ncol], [wb_.a()])
        col += ncol

    def stage_wst():
        S.op("sp", lambda: nc.sync.dma_start(out=wres_scr[:, :], in_=wres_flat.ap), r=[wres_flat.a()], w=[("wres_scr", 0, 1)], dma=True)
        ld("pool", w_out.ap, w_out_d[:, :, :], [w_out.a()])
        for c in range(8):
            wb = wstb[c % 2]
            for h in range(4):
                ld("pool", wb.ap[:, 10 * h:10 * h + 10, :], wst_d[c, :, 1280 * h:1280 * (h + 1)].rearrange("p (a b) -> p a b", b=128), [wb.a(10 * h, 10 * h + 10)])
            S.op("sp", lambda c=c, wb=wb: nc.sync.dma_start(out=wst_scr[c].rearrange("p (a b) -> p a b", b=128), in_=wb.ap),
                 r=[wb.a()], w=[("wst_scr", c, c + 1)], dma=True)

    A.off = mark_bcd
    wkv = A.buf("wkv", [8, D], BF16)
    memx = A.buf("memx", [D], F32)
    memb = A.buf("memb", [D], BF16)
    memT = A.buf("memT", [8, 256], BF16)
    ld("pool", wkv.ap, w_memkv_d[:, :, :], [wkv.a()])
    for s in range(2):
        for mb in range(2):
            ld("sp", memx.ap, mem[s, mb * 128:(mb + 1) * 128, :], [memx.a()])
            S.op("act", lambda: nc.scalar.copy(out=memb.ap, in_=memx.ap), r=[memx.a()], w=[memb.a()])
            ps = psum()
            for k in range(8):
                S.op("pe", lambda k=k, ps=ps: nc.tensor.transpose(ps.b[:, k * 128:(k + 1) * 128], memb.ap[:, k * 128:(k + 1) * 128], ident_b.ap),
                     r=[memb.a(), ident_b.a()], w=[ps.acc])
            S.op("dve", lambda ps=ps, mb=mb: nc.vector.tensor_copy(out=memT.ap[:, :, mb * 128:(mb + 1) * 128],
                                                                     in_=ps.b[:, :].rearrange("p (a b) -> p a b", b=128)),
                 r=[ps.acc], w=[memT.a()])
        for hm in range(4):
            ps = psum()
            mm_group(ps.f[:, 0:256], [(wkv.ap[:, k, hm * 128:(hm + 1) * 128], memT.ap[:, k, :]) for k in range(8)], ps.acc, [wkv.a(), memT.a()])
            S.op("act", lambda ps=ps, hm=hm, s=s: nc.scalar.copy(out=kmT[s].ap[:, hm, :], in_=ps.f[:, 0:256]), r=[ps.acc], w=[kmT[s].a(hm)])
        S.op("pool", lambda s=s: nc.gpsimd.memset(vmaug[s].ap[:, :, :, 128:129], 1.0), w=[vmaug[s].a()])
        for mc in range(2):
            ps = psum()
            mm_group(ps.f[:, :], [(memT.ap[:, k, mc * 128:(mc + 1) * 128], wkv.ap[:, k, 512:1024]) for k in range(8)], ps.acc, [wkv.a(), memT.a()])
            S.op("dve", lambda ps=ps, mc=mc, s=s: nc.vector.tensor_copy(out=vmaug[s].ap[:, mc, :, 0:128], in_=ps.f[:, :].rearrange("p (a b) -> p a b", b=128)),
                 r=[ps.acc], w=[vmaug[s].a(mc)])

    blk_counter = {"n": 0}
    pending = {}

    def supertile(ti):
        seg, st, nst = TILES[ti]
        row0 = SEGS[seg][0] + st * NT
        first, last = (st == 0), (st == nst - 1)
        if not (seg == 0 and st == 0):
            S.op("act", lambda: nc.scalar.dma_start(out=wres_flat.ap, in_=wres_scr[:, :]), r=[("wres_scr", 0, 1)], w=[wres_flat.a()], dma=True)
        def a1_load_stats(bw):
            xi = xin[bw % 4]
            if not (ti > 0 and bw < 4):
                ld("sp", xi.ap, xh[row0 + bw * 128: row0 + (bw + 1) * 128, :], [xi.a()])
            ln_stats(xi.ap, xi.a(), lnstat[bw % 2])

        a1_load_stats(0)
        pend_evac = None
        for bw in range(6):
            xi = xin[bw % 4]
            xb = xnb[bw % 2]
            center = 1 <= bw <= 4
            if bw + 1 < 6:
                a1_load_stats(bw + 1)
            if center:
                dst_ap, dst_acc = xn_c.ap[:, bw - 1, :], xn_c.a(bw - 1)
                ln_apply(xi.ap, xi.a(), dst_ap, dst_acc, ln_g_in, ln_b_in, lnstat[bw % 2])
                S.op("act", lambda dst_ap=dst_ap, xb=xb: nc.scalar.copy(out=xb.ap, in_=dst_ap), r=[dst_acc], w=[xb.a()])
            else:
                ln_apply2(xi.ap, xi.a(), xi.ap, xi.a(), xb.ap, xb.a(), ln_g_in, ln_b_in, lnstat[bw % 2])
            ps = psum()
            for k in range(8):
                S.op("pe", lambda k=k, ps=ps, xb=xb: nc.tensor.transpose(ps.b[:, k * 128:(k + 1) * 128], xb.ap[:, k * 128:(k + 1) * 128], ident_b.ap),
                     r=[xb.a(), ident_b.a()], w=[ps.acc])
            if pend_evac is not None:
                pend_evac()

            def mk(ps=ps, bw=bw):
                def f():
                    S.op("act", lambda: nc.scalar.copy(out=xnT.ap[:, :, bw * 128:(bw + 1) * 128], in_=ps.b[:, :].rearrange("p (a b) -> p a b", b=128)),
                         r=[ps.acc], w=[xnT.a()])
                return f
            pend_evac = mk()
        pend_evac()

        if pending.get("route") is not None:
            pending.pop("route")()
        if ti == 0:
            stage_wst()
        if ti + 1 < len(TILES):
            nseg, nst_, _ = TILES[ti + 1]
            nrow0 = SEGS[nseg][0] + nst_ * NT
            for bw in range(2):
                ld("sp", xin[bw].ap, xh[nrow0 + bw * 128: nrow0 + (bw + 1) * 128, :], [xin[bw].a()])

        def proj_fm(wbuf, nch, t0, t1, evac):
            for m in range(nch):
                tt = t0
                while tt < t1:
                    n = min(512, t1 - tt)
                    ps = psum()
                    mm_group(ps.f[:, 0:n], [(wbuf.ap[:, k, m * 128:(m + 1) * 128], xnT.ap[:, k, tt:tt + n]) for k in range(8)],
                             ps.acc, [wbuf.a(), xnT.a()])
                    evac(m, tt - t0, n, ps)
                    tt += n

        def ev_u(m, o, n, ps):
            S.op("act", lambda: nc.scalar.activation(out=uT.ap[:, m, o:o + n], in_=ps.f[:, 0:n], func=AF.Identity, bias=b_res.ap[:, m:m + 1]),
                 r=[ps.acc, b_res.a()], w=[uT.a(m)])
        proj_fm(w_u, 4, 112, 656, ev_u)
        def ev_q(m, o, n, ps):
            S.op("act", lambda: nc.scalar.activation(out=qT.ap[:, m, o:o + n], in_=ps.f[:, 0:n], func=AF.Identity, bias=bq8.ap[:, m:m + 1], scale=0.125),
                 r=[ps.acc, bq8.a()], w=[qT.a(m)])
        proj_fm(w_q, 8, 128, 640, ev_q)

        if first:
            S.op("dve", lambda: nc.vector.tensor_scalar(out=uT.ap[:, :, 0:16], in0=uT.ap[:, :, 0:16], scalar1=flg.ap[:, 2 * seg:2 * seg + 1], scalar2=None, op0=ALU.mult),
                 r=[uT.a(), flg.a()], w=[uT.a()])
        if last:
            S.op("dve", lambda: nc.vector.tensor_scalar(out=uT.ap[:, :, 528:544], in0=uT.ap[:, :, 528:544], scalar1=flg.ap[:, 2 * seg + 1:2 * seg + 2], scalar2=None, op0=ALU.mult),
                 r=[uT.a(), flg.a()], w=[uT.a()])
        for g in range(4):
            w_ = 2 << g
            h_ = w_ // 2
            u = uT.ap[:, g, :]
            src, src_acc = u, uT.a(g)
            m = 1
            bufs = [ptA, ptB]
            bi = 0
            while m < h_:
                dstb = bufs[bi]
                L = 544 - m
                S.op("dve", lambda src=src, dstb=dstb, m=m, L=L: nc.vector.tensor_tensor(out=dstb.ap[:, 0:L - m], in0=src[:, 0:L - m], in1=src[:, m:L], op=ALU.add),
                     r=[src_acc], w=[dstb.a()])
                src, src_acc = dstb.ap, dstb.a()
                m *= 2
                bi ^= 1
            dstb = bufs[bi]
            S.op("dve", lambda src=src, dstb=dstb, h_=h_: nc.vector.tensor_tensor(out=dstb.ap[:, 16:528], in0=src[:, 16 - h_:528 - h_], in1=src[:, 16:528], op=ALU.add),
                 r=[src_acc], w=[dstb.a()])
            S.op("dve", lambda dstb=dstb, g=g, w_=w_, u=u: nc.vector.scalar_tensor_tensor(out=pooledT.ap[:, g, :], in0=dstb.ap[:, 16:528], scalar=1.0 / w_, in1=u[:, 16:528],
                                                                                             op0=ALU.mult, op1=ALU.subtract),
                 r=[dstb.a(), uT.a(g)], w=[pooledT.a(g)])
            for (cond, e, c0) in ((first, 0, 0), (last, 1, 496)):
                if not cond:
                    continue
                ic = icn.ap[:, (seg * 2 + e) * 4 + g, :]
                S.op("dve", lambda dstb=dstb, ic=ic, c0=c0: nc.vector.tensor_tensor(out=dstb.ap[:, 16 + c0:32 + c0], in0=dstb.ap[:, 16 + c0:32 + c0], in1=ic, op=ALU.mult),
                     r=[dstb.a(), icn.a()], w=[dstb.a()])
                S.op("dve", lambda dstb=dstb, c0=c0, g=g, u=u: nc.vector.tensor_tensor(out=pooledT.ap[:, g, c0:c0 + 16], in0=dstb.ap[:, 16 + c0:32 + c0], in1=u[:, 16 + c0:32 + c0], op=ALU.subtract),
                     r=[dstb.a(), uT.a(g)], w=[pooledT.a(g)])
            ps = psum()
            mm_group(ps.f[:, :], [(w_pool.ap[:, g, :], pooledT.ap[:, g, :])], ps.acc, [w_pool.a(), pooledT.a(g)])
            S.op("act", lambda ps=ps, g=g: nc.scalar.activation(out=mixedT.ap[:, g, :], in_=ps.f[:, :], func=AF.Copy, scale=pscale.ap[:, g:g + 1]),
                 r=[ps.acc, pscale.a()], w=[mixedT.a(g)])

        def ev_k(m, o, n, ps):
            S.op("dve", lambda: nc.vector.tensor_scalar(out=kT.ap[:, m, o:o + n], in0=ps.f[:, 0:n], scalar1=b_res.ap[:, 12 + m:13 + m], scalar2=None, op0=ALU.add),
                 r=[ps.acc, b_res.a()], w=[kT.a(m)])
        proj_fm(w_k, 2, 0, 768, ev_k)
        S.op("act", lambda: nc.scalar.copy(out=vaug.ap[:, :, :, 64:65], in_=ones_f[:, 0:24].rearrange("p (a b c) -> p a b c", a=6, b=4)),
             r=[cst.a()], w=[vaug.a()])
        for bw in range(6):
            ps = psum()
            mm_group(ps.f[:, 0:256], [(xnT.ap[:, k, bw * 128:(bw + 1) * 128], w_v.ap[:, k, :]) for k in range(8)], ps.acc, [w_v.a(), xnT.a()])
            S.op("dve", lambda ps=ps, bw=bw: nc.vector.tensor_tensor(out=vaug.ap[:, bw, :, 0:64], in0=ps.f[:, 0:256].rearrange("p (a b) -> p a b", b=64),
                                                                      in1=bv.ap.rearrange("p (a b) -> p a b", b=64), op=ALU.add),
                 r=[ps.acc, bv.a()], w=[vaug.a(bw)])
        if first:
            S.op("dve", lambda: nc.vector.tensor_scalar(out=vaug.ap[:, 0], in0=vaug.ap[:, 0], scalar1=flg.ap[:, 2 * seg:2 * seg + 1], scalar2=None, op0=ALU.mult),
                 r=[vaug.a(0), flg.a()], w=[vaug.a(0)])
        if last:
            S.op("dve", lambda: nc.vector.tensor_scalar(out=vaug.ap[:, 5], in0=vaug.ap[:, 5], scalar1=flg.ap[:, 2 * seg + 1:2 * seg + 2], scalar2=None, op0=ALU.mult),
                 r=[vaug.a(5), flg.a()], w=[vaug.a(5)])

        units = [(qb, g) for qb in range(4) for g in range(4)]
        cstate = {}

        def c_scores(ui):
            qb, g = units[ui]
            pc = 64 * (g % 2)
            kc = g // 2
            pts = []
            for r_ in range(3):
                bwk = qb + r_
                ps = psum()
                psv = ps.f[:, :].rearrange("p (a b) -> p a b", b=128)
                S.op("pe", lambda ps=ps, psv=psv, bwk=bwk, pc=pc, kc=kc, qb=qb: nc.tensor.matmul(
                    psv, kT.ap[pc:pc + 64, kc, bwk * 128:(bwk + 1) * 128], qT.ap[pc:pc + 64, kc * 4:kc * 4 + 4, qb * 128:(qb + 1) * 128], start=True, stop=False),
                    r=[kT.a(kc), qT.a(kc * 4, kc * 4 + 4)], w=[ps.acc])
                S.op("pe", lambda ps=ps, psv=psv, r_=r_, g=g: nc.tensor.matmul(psv, ident_b.ap, biasT.ap[:, r_, 4 * g:4 * g + 4, :], start=False, stop=True),
                     r=[ident_b.a(), biasT.a(r_)], w=[ps.acc])
                pt = PT[(ui * 3 + r_) % 6]
                S.op("act", lambda ps=ps, pt=pt: nc.scalar.activation(out=pt.ap, in_=ps.f[:, :], func=AF.Exp), r=[ps.acc], w=[pt.a()])
                pts.append(pt)
            cstate[ui] = pts

        def c_out(ui):
            qb, g = units[ui]
            pts = cstate.pop(ui)
            osb = o_sb[qb % 2]
            dn = den[ui % 2]
            po = psum()
            for j in range(4):
                mm_group(po.f[:, j * 65:(j + 1) * 65],
                         [(pts[r_].ap[:, j * 128:(j + 1) * 128], vaug.ap[:, qb + r_, g, :]) for r_ in range(3)],
                         po.acc, [pts[0].a(), pts[1].a(), pts[2].a(), vaug.a(qb, qb + 3)])
            pov = po.f[:, 0:260].rearrange("p (a b) -> p a b", b=65)
            S.op("dve", lambda: nc.vector.tensor_tensor(out=dn.ap[:, 0:4], in0=pov[:, :, 64], in1=esink.ap[:, 4 * g:4 * g + 4], op=ALU.add),
                 r=[po.acc, esink.a()], w=[dn.a()])
            S.op("dve", lambda: nc.vector.reciprocal(out=dn.ap[:, 4:8], in_=dn.ap[:, 0:4]), r=[dn.a()], w=[dn.a()])
            S.op("dve", lambda: nc.vector.tensor_tensor(
                out=osb.ap[:, g * 256:(g + 1) * 256].rearrange("p (a b) -> p a b", b=64), in0=pov[:, :, 0:64],
                in1=dn.ap[:, 4:8].unsqueeze(2).to_broadcast([128, 4, 64]), op=ALU.mult),
                r=[po.acc, dn.a()], w=[osb.a(g * 256 // 1024, 1)] if False else [osb.a()])
            if g == 3:
                ps = psum()
                for k in range(8):
                    S.op("pe", lambda k=k, ps=ps: nc.tensor.transpose(ps.b[:, k * 128:(k + 1) * 128], osb.ap[:, k * 128:(k + 1) * 128], ident_b.ap),
                         r=[osb.a(), ident_b.a()], w=[ps.acc])
                S.op("act", lambda ps=ps: nc.scalar.copy(out=oT.ap[:, :, qb * 128:(qb + 1) * 128], in_=ps.b[:, :].rearrange("p (a b) -> p a b", b=128)),
                     r=[ps.acc], w=[oT.a()])

        c_scores(0)
        for ui in range(16):
            if ui + 1 < 16:
                c_scores(ui + 1)
            c_out(ui)

        def ev_qm(m, o, n, ps):
            S.op("act", lambda: nc.scalar.activation(out=qmT.ap[:, m, o:o + n], in_=ps.f[:, 0:n], func=AF.Identity, bias=b_res.ap[:, 16 + m:17 + m]),
                 r=[ps.acc, b_res.a()], w=[qmT.a(m)])
        proj_fm(w_qm, 4, 128, 640, ev_qm)
        for hm in range(4):
            for mc in range(2):
                ps = psum()
                mm_group(ps.f[:, :], [(kmT[seg].ap[:, hm, mc * 128:(mc + 1) * 128], qmT.ap[:, hm, :])], ps.acc, [kmT[seg].a(hm), qmT.a(hm)])
                S.op("act", lambda ps=ps, hm=hm, mc=mc: nc.scalar.activation(out=PmT.ap[:, hm * 2 + mc, :], in_=ps.f[:, :], func=AF.Exp, scale=128.0 ** -0.5),
                     r=[ps.acc], w=[PmT.a(hm * 2 + mc)])
        for qb in range(4):
            omb = om[qb % 2]
            dd = dend[qb % 2]
            for hh in range(2):
                po = psum()
                for j in range(2):
                    hm = hh * 2 + j
                    mm_group(po.f[:, j * 129:(j + 1) * 129],
                             [(PmT.ap[:, hm * 2 + mc, qb * 128:(qb + 1) * 128], vmaug[seg].ap[:, mc, hm, :]) for mc in range(2)],
                             po.acc, [PmT.a(hm * 2, hm * 2 + 2), vmaug[seg].a()])
                pov = po.f[:, 0:258].rearrange("p (a b) -> p a b", b=129)
                S.op("dve", lambda pov=pov, dd=dd: nc.vector.reciprocal(out=dd.ap[:, 0:2], in_=pov[:, :, 128]), r=[po.acc], w=[dd.a()])
                S.op("dve", lambda pov=pov, dd=dd, hh=hh, omb=omb: nc.vector.tensor_tensor(
                    out=omb.ap[:, hh * 256:(hh + 1) * 256].rearrange("p (a b) -> p a b", b=128), in0=pov[:, :, 0:128],
                    in1=dd.ap[:, 0:2].unsqueeze(2).to_broadcast([128, 2, 128]), op=ALU.mult),
                    r=[po.acc, dd.a()], w=[omb.a()])
            ps = psum()
            for k in range(4):
                S.op("pe", lambda k=k, ps=ps, omb=omb: nc.tensor.transpose(ps.b[:, k * 128:(k + 1) * 128], omb.ap[:, k * 128:(k + 1) * 128], ident_b.ap),
                     r=[omb.a(), ident_b.a()], w=[ps.acc])
            S.op("act", lambda ps=ps, qb=qb: nc.scalar.copy(out=omT.ap[:, :, qb * 128:(qb + 1) * 128], in_=ps.b[:, 0:512].rearrange("p (a b) -> p a b", b=128)),
                 r=[ps.acc], w=[omT.a()])

        gi = 0
        for c in range(8):
            wb = wstb[c % 2]
            S.op("sp", lambda c=c, wb=wb: nc.sync.dma_start(out=wb.ap, in_=wst_scr[c].rearrange("p (a b) -> p a b", b=128)),
                 r=[("wst_scr", c, c + 1)], w=[wb.a()], dma=True)
            mc_ = macc[c % 2]
            branches = [(24, 4, mixedT), (28, 8, oT), (36, 4, omT)]
            for r_ in range(3):
                pg = psum()
                mm_group(pg.f[:, :], [(wb.ap[:, r_ * 8 + k, :], xnT.ap[:, k, 128:640]) for k in range(8)], pg.acc, [wb.a(), xnT.a()])
                gs = gsig[gi % 3]
                gi += 1
                S.op("act", lambda pg=pg, gs=gs, r_=r_, c=c: nc.scalar.activation(out=gs.ap, in_=pg.f[:, :], func=AF.Sigmoid, bias=b_gate.ap[:, r_ * 8 + c:r_ * 8 + c + 1]),
                     r=[pg.acc, b_gate.a()], w=[gs.a()])
                s0, nk, src = branches[r_]
                pb = psum()
                mm_group(pb.f[:, :], [(wb.ap[:, s0 + k, :], src.ap[:, k, :]) for k in range(nk)], pb.acc, [wb.a(), src.a()])
                if r_ == 0:
                    S.op("dve", lambda gs=gs, pb=pb, mc_=mc_: nc.vector.tensor_tensor(out=mc_.ap, in0=gs.ap, in1=pb.f[:, :], op=ALU.mult),
                         r=[gs.a(), pb.acc], w=[mc_.a()])
                else:
                    S.op("dve", lambda gs=gs, pb=pb: nc.vector.tensor_tensor(out=gs.ap, in0=gs.ap, in1=pb.f[:, :], op=ALU.mult),
                         r=[gs.a(), pb.acc], w=[gs.a()])
                    if r_ == 1:
                        S.op("dve", lambda gs=gs, mc_=mc_: nc.vector.tensor_tensor(out=mc_.ap, in0=mc_.ap, in1=gs.ap, op=ALU.add),
                             r=[gs.a(), mc_.a()], w=[mc_.a()])
                    else:
                        S.op("dve", lambda gs=gs, mc_=mc_, c=c: nc.vector.tensor_tensor(out=mergedT.ap[:, c, :], in0=mc_.ap, in1=gs.ap, op=ALU.add),
                             r=[gs.a(), mc_.a()], w=[mergedT.a(c)])

        if ti + 1 < len(TILES):
            nseg, nst_, _ = TILES[ti + 1]
            nrow0 = SEGS[nseg][0] + nst_ * NT
            for bw in (2, 3):
                ld("sp", xin[bw].ap, xh[nrow0 + bw * 128: nrow0 + (bw + 1) * 128, :], [xin[bw].a()])
        blk0 = blk_counter["n"]
        blk_counter["n"] += 4
        groups = []
        for qb in range(4):
            for n in range(2):
                ps = psum()
                groups.append((qb, n, ps))
                for k in range(7):
                    S.op("pe", lambda ps=ps, k=k, qb=qb, n=n: nc.tensor.matmul(ps.f[:, :], mergedT.ap[:, k, qb * 128:(qb + 1) * 128], w_out.ap[:, k, n * 512:(n + 1) * 512],
                                                                             start=(k == 0), stop=False),
                         r=[mergedT.a(k), w_out.a(k)], w=[ps.acc])
        for (qb, n, ps) in groups:
            xo = x1[qb]
            S.op("pe", lambda ps=ps, qb=qb, n=n: nc.tensor.matmul(ps.f[:, :], mergedT.ap[:, 7, qb * 128:(qb + 1) * 128], w_out.ap[:, 7, n * 512:(n + 1) * 512],
                                                                 start=False, stop=True),
                 r=[mergedT.a(7), w_out.a(7)], w=[ps.acc])
            S.op("dve", lambda ps=ps, n=n, xo=xo, qb=qb: nc.vector.scalar_tensor_tensor(out=xo.ap[:, n * 512:(n + 1) * 512], in0=xn_c.ap[:, qb, n * 512:(n + 1) * 512],
                                                                                       scalar=ALPHA, in1=ps.f[:, :], op0=ALU.mult, op1=ALU.add),
                 r=[ps.acc, xn_c.a(qb)], w=[xo.a()])
        for qb in range(4):
            ln_stats(x1[qb].ap, x1[qb].a(), lnstatF[qb])
        for qb in range(4):
            blk = blk0 + qb
            xo, xob = x1[qb], x1b[qb]
            ln_apply(xo.ap, xo.a(), xo.ap, xo.a(), ln_g1, ln_b1, lnstatF[qb])
            S.op("sp", lambda xo=xo, blk=blk: nc.sync.dma_start(out=x1_scr[blk * 128:(blk + 1) * 128, :], in_=xo.ap),
                 r=[xo.a()], w=[("x1_scr", blk, blk + 1)], dma=True)
            S.op("act", lambda xo=xo, xob=xob: nc.scalar.copy(out=xob.ap, in_=xo.ap), r=[xo.a()], w=[xob.a()])

        def f_transposes(qb):
            xo = x1[qb]
            xt = x1T[qb % 2]
            for hh in range(2):
                ps = psum()
                for k in range(4):
                    kk = hh * 4 + k
                    S.op("pe", lambda k=k, kk=kk, ps=ps: nc.tensor.transpose(ps.f[:, k * 128:(k + 1) * 128], xo.ap[:, kk * 128:(kk + 1) * 128], ident_f),
                         r=[xo.a(), cst.a()], w=[ps.acc])
                S.op("act", lambda ps=ps, hh=hh: nc.scalar.copy(out=xt.ap[:, hh * 4:hh * 4 + 4, :], in_=ps.f[:, :].rearrange("p (a b) -> p a b", b=128)),
                     r=[ps.acc], w=[xt.a(hh * 4, hh * 4 + 4)])

        def f_logits(qb):
            xt = x1T[qb % 2]
            ps = psum()
            mm_group(ps.f[:, 0:36], [(xt.ap[:, k, :], wr.ap[:, k, :]) for k in range(8)], ps.acc, [xt.a(), wr.a()])
            S.op("dve", lambda: nc.vector.tensor_copy(out=lg_all.ap[:, qb, :], in_=ps.f[:, 0:36]), r=[ps.acc], w=[lg_all.a(qb)])

        f_transposes(0)
        for qb in range(4):
            if qb + 1 < 4:
                f_transposes(qb + 1)
            f_logits(qb)

        def route_and_scatter():
            router4(blk0)
            for qb in range(4):
                blk = blk0 + qb
                xob = x1b[qb]
                for kk in range(2):
                    S.op("pool", lambda xob=xob, blk=blk, kk=kk: nc.gpsimd.indirect_dma_start(
                        out=xs_scr[:, :], out_offset=bass.IndirectOffsetOnAxis(ap=idx_all.ap[:, blk, kk:kk + 1].bitcast(U32), axis=0),
                        in_=xob.ap, in_offset=None),
                        r=[xob.a(), idx_all.a(blk)], w=[("xs_scr", 0, NE * CAP)], dma=True)
        if ti + 1 < len(TILES):
            pending["route"] = route_and_scatter
        else:
            route_and_scatter()

    def router4(blk0):
        rows = {}

        BIG = ("elm", "sel", "oh1", "oh2", "esh", "ee", "wraw", "wmat", "t1", "t2", "dest", "ovf", "tr", "t3", "t4")
        nbig = {"n": 0}
        nsmall = {"n": 0}

        def row(name):
            if name not in rows:
                if name in BIG:
                    rows[name] = (nbig["n"] * 128, 128)
                    nbig["n"] += 1
                    assert nbig["n"] <= 15
                else:
                    rows[name] = (15 * 128 + nsmall["n"] * 32, 32)
                    nsmall["n"] += 1
                    assert nsmall["n"] <= 15
            off, n = rows[name]
            return rbig.ap[:, off:off + n], ("arena", rbig.lo + off * 4, rbig.lo + (off + n) * 4)

        def v3(ap, n):
            return ap[:, 0:4 * n].rearrange("p (a b) -> p a b", b=n)

        def dv(fn, r, w):
            S.op("dve", fn, r=r, w=w)

        def bc(ap1, n):
            return ap1.to_broadcast([128, 4, n])

        lg = lg_all.ap
        lg_a = lg_all.a()
        gl = lg[:, :, 0:4]
        r_, gmax_a = row("gmax"); gmax = r_[:, 0:4]
        dv(lambda: nc.vector.tensor_reduce(out=gmax, in_=gl, axis=AX.X, op=ALU.max), [lg_a], [gmax_a])
        r_, ohg_a = row("ohg"); ohg = v3(r_, 4)
        dv(lambda: nc.vector.tensor_tensor(out=ohg, in0=gl, in1=bc(gmax.unsqueeze(2), 4), op=ALU.is_equal), [lg_a, gmax_a], [ohg_a])
        r_, gsh_a = row("gsh"); gsh = v3(r_, 4)
        dv(lambda: nc.vector.tensor_tensor(out=gsh, in0=gl, in1=bc(gmax.unsqueeze(2), 4), op=ALU.subtract), [lg_a, gmax_a], [gsh_a])
        r_, ge_a = row("ge"); ge = v3(r_, 4)
        S.op("act", lambda: nc.scalar.activation(out=ge, in_=gsh, func=AF.Exp), r=[gsh_a], w=[ge_a])
        r_, gsum_a = row("gsum"); gsum = r_[:, 0:4]
        dv(lambda: nc.vector.tensor_reduce(out=gsum, in_=ge, axis=AX.X, op=ALU.add), [ge_a], [gsum_a])
        r_, pen_a = row("pen"); pen = v3(r_, 4)
        dv(lambda: nc.vector.tensor_scalar(out=pen, in0=ohg, scalar1=1e9, scalar2=-1e9, op0=ALU.mult, op1=ALU.add), [ohg_a], [pen_a])
        r_, elm_a = row("elm"); elm = v3(r_, 32)
        for b in range(4):
            dv(lambda b=b: nc.vector.tensor_tensor(out=elm[:, b, :].rearrange("p (a b) -> p a b", b=8), in0=lg[:, b, 4:36].rearrange("p (a b) -> p a b", b=8),
                                                   in1=pen[:, b, :].unsqueeze(2).to_broadcast([128, 4, 8]), op=ALU.add), [lg_a, pen_a], [elm_a])
        r_, m8_a = row("m8"); m8 = v3(r_, 8)
        for b in range(4):
            dv(lambda b=b: nc.vector.max(out=m8[:, b, :], in_=elm[:, b, :]), [elm_a], [m8_a])
        top1 = m8[:, :, 0:1]
        top2 = m8[:, :, 1:2]
        r_, sel_a = row("sel"); sel = v3(r_, 32); sel_flat = r_[:, 0:128]
        dv(lambda: nc.vector.tensor_tensor(out=sel, in0=elm, in1=bc(top2, 32), op=ALU.is_ge), [elm_a, m8_a], [sel_a])
        r_, oh1_a = row("oh1"); oh1 = v3(r_, 32)
        dv(lambda: nc.vector.tensor_tensor(out=oh1, in0=elm, in1=bc(top1, 32), op=ALU.is_equal), [elm_a, m8_a], [oh1_a])
        r_, oh2_a = row("oh2"); oh2 = v3(r_, 32)
        dv(lambda: nc.vector.tensor_tensor(out=oh2, in0=sel, in1=oh1, op=ALU.subtract), [sel_a, oh1_a], [oh2_a])
        r_, esh_a = row("esh"); esh = v3(r_, 32)
        dv(lambda: nc.vector.tensor_tensor(out=esh, in0=elm, in1=bc(top1, 32), op=ALU.subtract), [elm_a, m8_a], [esh_a])
        dv(lambda: nc.vector.tensor_scalar(out=esh, in0=esh, scalar1=-80.0, scalar2=None, op0=ALU.max), [esh_a], [esh_a])
        r_, ee_a = row("ee"); ee = v3(r_, 32)
        S.op("act", lambda: nc.scalar.activation(out=ee, in_=esh, func=AF.Exp), r=[esh_a], w=[ee_a])
        r_, wraw_a = row("wraw"); wraw = v3(r_, 32)
        dv(lambda: nc.vector.tensor_tensor(out=wraw, in0=sel, in1=ee, op=ALU.mult), [sel_a, ee_a], [wraw_a])
        r_, ssum_a = row("ssum"); ssum = r_[:, 0:4]
        dv(lambda: nc.vector.tensor_reduce(out=ssum, in_=wraw, axis=AX.X, op=ALU.add), [wraw_a], [ssum_a])
        dv(lambda: nc.vector.tensor_tensor(out=ssum, in0=ssum, in1=gsum, op=ALU.mult), [ssum_a, gsum_a], [ssum_a])
        r_, coef_a = row("coef"); coef = r_[:, 0:4]
        dv(lambda: nc.vector.reciprocal(out=coef, in_=ssum), [ssum_a], [coef_a])
        r_, wmat_a = row("wmat"); wmat = v3(r_, 32)
        dv(lambda: nc.vector.tensor_tensor(out=wmat, in0=wraw, in1=bc(coef.unsqueeze(2), 32), op=ALU.mult), [wraw_a, coef_a], [wmat_a])
        r_, w12_a = row("w12"); w12 = r_[:, 0:8].rearrange("p (a b) -> p a b", b=4)
        r_, t1_a = row("t1"); t1 = v3(r_, 32)
        dv(lambda: nc.vector.tensor_tensor(out=t1, in0=wmat, in1=oh1, op=ALU.mult), [wmat_a, oh1_a], [t1_a])
        dv(lambda: nc.vector.tensor_reduce(out=w12[:, 0, :], in_=t1, axis=AX.X, op=ALU.add), [t1_a], [w12_a])
        r_, t2_a = row("t2"); t2 = v3(r_, 32)
        dv(lambda: nc.vector.tensor_tensor(out=t2, in0=wmat, in1=oh2, op=ALU.mult), [wmat_a, oh2_a], [t2_a])
        dv(lambda: nc.vector.tensor_reduce(out=w12[:, 1, :], in_=t2, axis=AX.X, op=ALU.add), [t2_a], [w12_a])
        pr = psum()
        mm_group(pr.f[:, 0:128], [(triU, sel_flat)], pr.acc, [cst.a(), sel_a])
        mm_group(pr.f[:, 128:256], [(ones_f, sel_flat)], pr.acc, [cst.a(), sel_a])
        r_, dest_a = row("dest"); dest = v3(r_, 32)
        for b in range(4):
            dv(lambda b=b: nc.vector.tensor_tensor(out=dest[:, b, :], in0=pr.f[:, 32 * b:32 * b + 32], in1=carry.ap, op=ALU.add), [pr.acc, carry.a()], [dest_a])
            dv(lambda b=b: nc.vector.tensor_tensor(out=carry.ap, in0=pr.f[:, 128 + 32 * b:128 + 32 * b + 32], in1=carry.ap, op=ALU.add), [pr.acc, carry.a()], [carry.a()])
        r_, ovf_a = row("ovf"); ovf = v3(r_, 32)
        dv(lambda: nc.vector.tensor_scalar(out=ovf, in0=dest, scalar1=float(CAP), scalar2=None, op0=ALU.is_ge), [dest_a], [ovf_a])
        dv(lambda: nc.vector.tensor_tensor(out=dest, in0=dest, in1=ebase.unsqueeze(1).to_broadcast([128, 4, 32]), op=ALU.add), [dest_a, cst.a()], [dest_a])
        r_, tr_a = row("tr"); tr = v3(r_, 32)
        dv(lambda: nc.vector.tensor_scalar(out=tr, in0=dest, scalar1=-1.0, scalar2=trashcol, op0=ALU.mult, op1=ALU.add), [dest_a, cst.a()], [tr_a])
        dv(lambda: nc.vector.tensor_tensor(out=tr, in0=tr, in1=ovf, op=ALU.mult), [tr_a, ovf_a], [tr_a])
        dv(lambda: nc.vector.tensor_tensor(out=dest, in0=dest, in1=tr, op=ALU.add), [dest_a, tr_a], [dest_a])
        r_, d12_a = row("d12"); d12 = r_[:, 0:8].rearrange("p (a b) -> p a b", b=4)
        r_, t3_a = row("t3"); t3 = v3(r_, 32)
        dv(lambda: nc.vector.tensor_tensor(out=t3, in0=dest, in1=oh1, op=ALU.mult), [dest_a, oh1_a], [t3_a])
        dv(lambda: nc.vector.tensor_reduce(out=d12[:, 0, :], in_=t3, axis=AX.X, op=ALU.add), [t3_a], [d12_a])
        r_, t4_a = row("t4"); t4 = v3(r_, 32)
        dv(lambda: nc.vector.tensor_tensor(out=t4, in0=dest, in1=oh2, op=ALU.mult), [dest_a, oh2_a], [t4_a])
        dv(lambda: nc.vector.tensor_reduce(out=d12[:, 1, :], in_=t4, axis=AX.X, op=ALU.add), [t4_a], [d12_a])
        r_, okm_a = row("okm"); okm = r_[:, 0:8].rearrange("p (a b) -> p a b", b=4)
        dv(lambda: nc.vector.tensor_scalar(out=okm, in0=d12, scalar1=float(NE * CAP) - 0.5, scalar2=None, op0=ALU.is_lt), [d12_a], [okm_a])
        dv(lambda: nc.vector.tensor_tensor(out=w12, in0=w12, in1=okm, op=ALU.mult), [w12_a, okm_a], [w12_a])
        for kk in range(2):
            dv(lambda kk=kk: nc.vector.tensor_copy(out=wts_all.ap[:, blk0:blk0 + 4, kk], in_=w12[:, kk, :]), [w12_a], [wts_all.a(blk0, blk0 + 4)])
            dv(lambda kk=kk: nc.vector.tensor_copy(out=idx_all.ap[:, blk0:blk0 + 4, kk], in_=d12[:, kk, :]), [d12_a], [idx_all.a(blk0, blk0 + 4)])

    TILES = [(seg, st, SEGS[seg][1] // NT) for seg in range(2) for st in range(SEGS[seg][1] // NT)]
    for ti in range(len(TILES)):
        supertile(ti)

    A.off = mark_phase
    ln_g2 = A.buf("ln_g2", [D], F32)
    ln_b2 = A.buf("ln_b2", [D], F32)
    ld("sp", ln_g2.ap, ln_d[4], [ln_g2.a()])
    ld("sp", ln_b2.ap, ln_d[5], [ln_b2.a()])
    mark_p2 = A.off
    wgu = [A.buf(f"wgu{i}", [8, D], BF16) for i in range(3)]
    wdn = [A.buf(f"wdn{i}", [4, D], BF16) for i in range(3)]
    xg = [A.buf(f"xg{i}", [4, D], BF16) for i in range(2)]
    XT = A.buf("XT", [8, 512], BF16)
    sgb = [A.buf(f"sg{i}", [512], F32) for i in range(2)]
    hT = A.buf("hT", [4, 512], BF16)
    yst = [A.buf(f"yst{i}", [4, D], F32) for i in range(2)]

    def load_expert_w(e):
        wg, wd_ = wgu[e % 3], wdn[e % 3]
        ld("pool", wg.ap, w_gu_d[e].rearrange("(c p) n -> p c n", p=128), [wg.a()])
        ld("pool", wd_.ap, w_down_d[e].rearrange("(c p) n -> p c n", p=128), [wd_.a()])

    def load_xg(e):
        xg_ = xg[e % 2]
        S.op("sp", lambda: nc.sync.dma_start(out=xg_.ap, in_=xs_scr[e * CAP:(e + 1) * CAP, :].rearrange("(b p) d -> p b d", p=128)),
             r=[("xs_scr", 0, NE * CAP)], w=[xg_.a()], dma=True)

    load_expert_w(0)
    load_expert_w(1)
    load_xg(0)
    for e in range(NE):
        wg, wd_, xg_, ys_ = wgu[e % 3], wdn[e % 3], xg[e % 2], yst[e % 2]
        if e + 2 < NE:
            load_expert_w(e + 2)
        if e + 1 < NE:
            load_xg(e + 1)
        for i in range(4):
            ps = psum()
            for kk in range(2):
                k = 2 * i + kk
                for b in range(4):
                    S.op("pe", lambda k=k, kk=kk, b=b, ps=ps, xg_=xg_: nc.tensor.transpose(ps.b[:, kk * 512 + b * 128: kk * 512 + (b + 1) * 128],
                                                                                           xg_.ap[:, b, k * 128:(k + 1) * 128], ident_b.ap),
                         r=[xg_.a(), ident_b.a()], w=[ps.acc])
            eng = "act" if i % 2 == 0 else "dve"
            if eng == "act":
                S.op("act", lambda ps=ps, i=i: nc.scalar.copy(out=XT.ap[:, 2 * i:2 * i + 2, :], in_=ps.b[:, :].rearrange("p (a b) -> p a b", b=512)),
                     r=[ps.acc], w=[XT.a(2 * i, 2 * i + 2)])
            else:
                S.op("dve", lambda ps=ps, i=i: nc.vector.tensor_copy(out=XT.ap[:, 2 * i:2 * i + 2, :], in_=ps.b[:, :].rearrange("p (a b) -> p a b", b=512)),
                     r=[ps.acc], w=[XT.a(2 * i, 2 * i + 2)])
        for m in range(4):
            pg = psum()
            mm_group(pg.f[:, :], [(wg.ap[:, k, m * 128:(m + 1) * 128], XT.ap[:, k, :]) for k in range(8)], pg.acc, [wg.a(), XT.a()])
            pu = psum()
            mm_group(pu.f[:, :], [(wg.ap[:, k, 512 + m * 128:512 + (m + 1) * 128], XT.ap[:, k, :]) for k in range(8)], pu.acc, [wg.a(), XT.a()])
            sg = sgb[m % 2]
            S.op("act", lambda pg=pg, sg=sg: nc.scalar.activation(out=sg.ap, in_=pg.f[:, :], func=AF.Silu), r=[pg.acc], w=[sg.a()])
            S.op("dve", lambda pu=pu, sg=sg, m=m: nc.vector.tensor_tensor(out=hT.ap[:, m, :], in0=sg.ap, in1=pu.f[:, :], op=ALU.mult),
                 r=[pu.acc, sg.a()], w=[hT.a(m)])
        for b in range(4):
            for n in range(2):
                ps = psum()
                mm_group(ps.f[:, :], [(hT.ap[:, j, b * 128:(b + 1) * 128], wd_.ap[:, j, n * 512:(n + 1) * 512]) for j in range(4)], ps.acc, [hT.a(), wd_.a()])
                if n == 0:
                    S.op("act", lambda ps=ps, b=b, n=n, ys_=ys_: nc.scalar.copy(out=ys_.ap[:, b, n * 512:(n + 1) * 512], in_=ps.f[:, :]), r=[ps.acc], w=[ys_.a(b)])
                else:
                    S.op("dve", lambda ps=ps, b=b, n=n, ys_=ys_: nc.vector.tensor_copy(out=ys_.ap[:, b, n * 512:(n + 1) * 512], in_=ps.f[:, :]), r=[ps.acc], w=[ys_.a(b)])
        S.op("sp", lambda e=e, ys_=ys_: nc.sync.dma_start(out=ys_scr[e * CAP:(e + 1) * CAP, :].rearrange("(b p) d -> p b d", p=128), in_=ys_.ap),
             r=[ys_.a()], w=[("ys_scr", e, e + 1)], dma=True)

    A.off = mark_p2
    NB3 = 4
    ya = [A.buf(f"ya{i}", [D], F32) for i in range(NB3)]
    yb = [A.buf(f"yb{i}", [D], F32) for i in range(NB3)]
    x1r = [A.buf(f"x1r{i}", [D], F32) for i in range(NB3)]
    z2 = [A.buf(f"z2{i}", [D], F32) for i in range(4)]
    outb = [A.buf(f"outb{i}", [D], F32) for i in range(3)]
    lnstat2 = [A.buf(f"lnstat2{i}", [16], F32) for i in range(4)]

    def p3_loads(blk):
        i = blk % NB3
        for kk, dst in ((0, ya[i]), (1, yb[i])):
            S.op("pool", lambda dst=dst, kk=kk: nc.gpsimd.indirect_dma_start(
                out=dst.ap, out_offset=None, in_=ys_scr[:, :],
                in_offset=bass.IndirectOffsetOnAxis(ap=idx_all.ap[:, blk, kk:kk + 1].bitcast(U32), axis=0)),
                r=[("ys_scr", 0, NE + 1), idx_all.a(blk)], w=[dst.a()], dma=True)
        S.op("act", lambda: nc.scalar.dma_start(out=x1r[i].ap, in_=x1_scr[blk * 128:(blk + 1) * 128, :]),
             r=[("x1_scr", blk, blk + 1)], w=[x1r[i].a()], dma=True)

    for bb in ya + yb:
        S.op("pool", lambda bb=bb: nc.gpsimd.memset(bb.ap, 0.0), w=[bb.a()])
    PD = NB3 - 1
    for b_ in range(PD):
        p3_loads(b_)

    def p3_finish_a(blk):
        j = blk % 4
        st_ = lnstat2[j]
        S.op("dve", lambda: nc.vector.tensor_scalar(out=st_.ap[:, 11:12], in0=st_.ap[:, 12:13], scalar1=st_.ap[:, 15:16], scalar2=-1.0, op0=ALU.mult, op1=ALU.mult),
             r=[st_.a(0)], w=[st_.a(0)])
        S.op("act", lambda: nc.scalar.activation(out=z2[j].ap, in_=z2[j].ap, func=AF.Identity, bias=st_.ap[:, 11:12], scale=st_.ap[:, 15:16]),
             r=[z2[j].a(), st_.a(0)], w=[z2[j].a()])

    def p3_finish_b(blk):
        j = blk % 4
        k = blk % 3
        S.op("dve", lambda: nc.vector.tensor_tensor(out=z2[j].ap, in0=z2[j].ap, in1=ln_g2.ap, op=ALU.mult), r=[z2[j].a(), ln_g2.a()], w=[z2[j].a()])
        S.op("dve", lambda: nc.vector.tensor_tensor(out=outb[k].ap, in0=z2[j].ap, in1=ln_b2.ap, op=ALU.add), r=[z2[j].a(), ln_b2.a()], w=[outb[k].a()])
        S.op("sp", lambda: nc.sync.dma_start(out=y_out[blk * 128:(blk + 1) * 128, :], in_=outb[k].ap),
             r=[outb[k].a()], w=[("y_out", blk, blk + 1)], dma=True)

    for blk in range(NBLK):
        if blk + PD < NBLK:
            p3_loads(blk + PD)
        i = blk % NB3
        j = blk % 4
        S.op("act", lambda i=i: nc.scalar.mul(out=x1r[i].ap, in_=x1r[i].ap, mul=ALPHA), r=[x1r[i].a()], w=[x1r[i].a()])
        S.op("dve", lambda i=i, j=j, blk=blk: nc.vector.scalar_tensor_tensor(out=z2[j].ap, in0=ya[i].ap, scalar=wts_all.ap[:, blk, 0:1], in1=x1r[i].ap, op0=ALU.mult, op1=ALU.add),
             r=[ya[i].a(), x1r[i].a(), wts_all.a(blk)], w=[z2[j].a()])
        S.op("dve", lambda i=i, j=j, blk=blk: nc.vector.scalar_tensor_tensor(out=z2[j].ap, in0=yb[i].ap, scalar=wts_all.ap[:, blk, 1:2], in1=z2[j].ap, op0=ALU.mult, op1=ALU.add),
             r=[yb[i].a(), z2[j].a(), wts_all.a(blk)], w=[z2[j].a()])
        ln_stats(z2[j].ap, z2[j].a(), lnstat2[j])
        if blk >= 1:
            p3_finish_a(blk - 1)
        if blk >= 2:
            p3_finish_b(blk - 2)
    p3_finish_a(NBLK - 1)
    p3_finish_b(NBLK - 2)
    p3_finish_b(NBLK - 1)

    sems_c = {e: nc.alloc_semaphore(f"s_{e}") for e in Sched.COMPUTE}
    sems_d = {"sp": [nc.alloc_semaphore(f"d_sp{i}") for i in range(24)],
              "pool": [nc.alloc_semaphore(f"d_pool{i}") for i in range(24)],
              "act": [nc.alloc_semaphore(f"d_act{i}") for i in range(8)]}
    nw = S.emit(sems_c, sems_d)
    print(f"[kernel] ops={len(S.ops)} waits={nw}")
    return nc


def _t5_bucket_np(rel):
    nb = 16
    max_exact = 8
    ret = np.where(rel > 0, nb, 0)
    n = np.abs(rel)
    nf = np.maximum(n, 1).astype(np.float32)
    large = max_exact + (np.log(nf / max_exact) / math.log(128 / max_exact) * (nb - max_exact)).astype(np.int32)
    large = np.minimum(large, nb - 1)
    return ret + np.where(n < max_exact, n, large)


_NC_CACHE = {}


def kernel(x_prompt, x_sample, mem_prompt, mem_sample, ln_in_g, ln_in_b, rel_bias_table, w_in, b_in,
           w_pool, pool_scale, p_pool, sink, p_attn, w_mem_kv, p_mem, w_out, ln1_g, ln1_b,
           w_router_group, w_router_expert, w_gu, w_down, ln2_g, ln2_b):
    f32 = np.float32
    A_ = lambda a: np.ascontiguousarray(np.asarray(a, dtype=f32))
    x_prompt, x_sample, mem_prompt, mem_sample = map(A_, (x_prompt, x_sample, mem_prompt, mem_sample))
    w_in0 = A_(w_in)[0]
    b_in0 = A_(b_in)[0]
    o1, o2, o3, o4, o5 = 512, 1536, 1792, 2048, 2560

    qcols = []
    for gg in range(2):
        for j in range(4):
            for g_ in (2 * gg, 2 * gg + 1):
                h = 4 * g_ + j
                qcols.extend(range(o1 + 64 * h, o1 + 64 * h + 64))
    res_cols = list(range(0, 512)) + qcols + list(range(o2, o3)) + list(range(o3, o4)) + list(range(o4, o5))
    res_cols = np.array(res_cols)
    kmaj = lambda w: np.ascontiguousarray(w.reshape(w.shape[0] // 128, 128, w.shape[1]).transpose(1, 0, 2))
    w_res = kmaj(w_in0[:, res_cols])
    b_res_full = b_in0[res_cols]
    bcols = np.concatenate([b_res_full[0:512], b_res_full[512:1536], b_res_full[1536:1792], b_res_full[2048:2560]])
    b_res = np.zeros((128, 20), f32)
    b_res[:, 0:14] = bcols[0:1792].reshape(14, 128).T
    b_res[:, 16:20] = bcols[1792:2304].reshape(4, 128).T
    bv_bc = np.ascontiguousarray(np.broadcast_to(b_res_full[1792:2048][None, :], (128, 256)))
    b_gate = np.ascontiguousarray(b_in0[o5:].reshape(24, 128).T)
    wg_k = kmaj(w_in0[:, o5:])
    pp_k = kmaj(A_(p_pool)[0])
    pa_k = kmaj(A_(p_attn)[0])
    pm_k = kmaj(A_(p_mem)[0])
    wst = np.empty((8, 128, 40, 128), f32)
    for c in range(8):
        for r in range(3):
            wst[c, :, r * 8:(r + 1) * 8, :] = wg_k[:, :, r * 1024 + c * 128: r * 1024 + (c + 1) * 128]
        wst[c, :, 24:28, :] = pp_k[:, :, c * 128:(c + 1) * 128]
        wst[c, :, 28:36, :] = pa_k[:, :, c * 128:(c + 1) * 128]
        wst[c, :, 36:40, :] = pm_k[:, :, c * 128:(c + 1) * 128]
    wst = wst.reshape(8, 128, 5120)
    w_out_l = kmaj(A_(w_out)[0])
    w_pool_l = np.ascontiguousarray(A_(w_pool)[0].transpose(1, 0, 2))
    pscale = np.ascontiguousarray(A_(pool_scale)[0].reshape(4, 128).T)
    w_memkv_l = kmaj(A_(w_mem_kv)[0])
    bc = lambda v: np.broadcast_to(np.asarray(v, f32).reshape(1, -1), (128, np.asarray(v).size))
    ln_bc = np.ascontiguousarray(np.stack([bc(ln_in_g), bc(ln_in_b), bc(A_(ln1_g)[0]), bc(A_(ln1_b)[0]), bc(A_(ln2_g)[0]), bc(A_(ln2_b)[0])]))
    kk = np.arange(128)[:, None, None]
    rr = np.arange(3)[None, :, None]
    qq = np.arange(128)[None, None, :]
    rel = (rr - 1) * 128 + kk - qq
    bidx = _t5_bucket_np(rel)
    tab = A_(rel_bias_table)
    biasT = np.ascontiguousarray(tab[bidx].transpose(0, 1, 3, 2))
    sink_bc = np.ascontiguousarray(bc(A_(sink)[0]))
    wr_l = kmaj(np.concatenate([A_(w_router_group)[0], A_(w_router_expert)[0]], axis=1))
    consts = np.zeros((128, 419), f32)
    consts[:, 0:128] = np.eye(128, dtype=f32)
    consts[:, 128:256] = np.triu(np.ones((128, 128), f32), k=1)
    consts[:, 256:384] = 1.0
    consts[:, 384:416] = (np.arange(32) * CAP)[None, :]
    consts[:, 416] = -0.5
    consts[:, 417] = LN_EPS
    consts[:, 418] = NE * CAP + np.arange(128)
    w_gu0 = A_(w_gu)[0]
    w_down0 = A_(w_down)[0]

    in_maps = []
    for c in range(NCORES):
        b, half = c // 2, c % 2
        xh = np.zeros((XH_ROWS, D), f32)
        flags = np.ones((128, 4), f32)
        invc = np.empty((128, 2, 2, 4, 16), f32)
        for s, (xsrc, ntok) in enumerate(((x_prompt, P_TOK), (x_sample, S_TOK))):
            Sfull = 2 * ntok
            t0 = half * ntok
            lo, hi = t0 - 128, t0 + ntok + 128
            clo, chi = max(lo, 0), min(hi, Sfull)
            base = SEGS[s][0]
            xh[base + (clo - lo): base + (chi - lo)] = xsrc[b, clo:chi]
            flags[:, 2 * s] = 1.0 if lo >= 0 else 0.0
            flags[:, 2 * s + 1] = 1.0 if hi <= Sfull else 0.0
            for g_ in range(4):
                w_ = 2 << g_
                for e_, tpos in ((0, t0 + np.arange(16)), (1, t0 + ntok - 16 + np.arange(16))):
                    cnt = np.minimum(tpos + w_ // 2, Sfull) - np.maximum(tpos - w_ // 2, 0)
                    invc[:, s, e_, g_, :] = (1.0 / cnt.astype(np.float64)).astype(f32)[None, :]
        in_maps.append({
            "xh": xh, "mem": np.ascontiguousarray(np.stack([mem_prompt[b], mem_sample[b]])), "flags": flags, "invcnt": invc,
            "w_res": w_res, "b_res": b_res, "bv_bc": bv_bc, "b_gate": b_gate, "wst": wst, "w_out_l": w_out_l,
            "w_pool_l": w_pool_l, "pscale": pscale, "w_memkv_l": w_memkv_l, "ln_bc": ln_bc, "biasT": biasT,
            "sink_bc": sink_bc, "wr_l": wr_l, "w_gu": w_gu0, "w_down": w_down0, "consts": consts,
        })
    if "nc" not in _NC_CACHE:
        _NC_CACHE["nc"] = build_program()
    nc = _NC_CACHE["nc"]
    res = run_bass_kernel_spmd(nc, in_maps, core_ids=list(range(NCORES)))
    y_p = np.empty((4, 8192, D), f32)
    y_s = np.empty((4, 4096, D), f32)
    for c in range(NCORES):
        b, half = c // 2, c % 2
        y = res.results[c]["y"]
        y_p[b, half * P_TOK:(half + 1) * P_TOK] = y[:P_TOK]
        y_s[b, half * S_TOK:(half + 1) * S_TOK] = y[P_TOK:]
    return (y_p, y_s)
```

```python
import numpy as np
import math
import concourse.bass as bass
import concourse.mybir as mybir
from concourse.bass_utils import run_bass_kernel_spmd

F32 = mybir.dt.float32
BF16 = mybir.dt.bfloat16
I32 = mybir.dt.int32
U32 = mybir.dt.uint32
AF = mybir.ActivationFunctionType
ALU = mybir.AluOpType
AX = mybir.AxisListType

D = 1024
NCORES = 8
P_TOK = 4096
S_TOK = 2048
TOK = P_TOK + S_TOK
NBLK = TOK // 128
NT = 512
CAP = 512
NE = 32
ALPHA = 2.0 ** 0.25
LN_EPS = 1e-5
NEG = -30000.0
SEGS = [(0, P_TOK), (P_TOK + 256, S_TOK)]
XH_ROWS = P_TOK + 256 + S_TOK + 256
C_U, C_Q, C_K, C_V, C_QM, C_RES = 0, 512, 1536, 1792, 2048, 2560

DSIZE = {F32: 4, BF16: 2, I32: 4, U32: 4}


class Buf:
    def __init__(self, name, arena, lo, shape, dtype, ap):
        self.name, self.arena, self.lo, self.shape, self.dtype, self.ap = name, arena, lo, tuple(shape), dtype, ap
        n = 1
        for s in shape:
            n *= s
        self.nbytes = n * DSIZE[dtype]
        self.hi = lo + self.nbytes
        self.stride0 = self.nbytes // shape[0]

    def a(self, i0=None, i1=None):
        if i0 is None:
            return (self.arena, self.lo, self.hi)
        if i1 is None:
            i1 = i0 + 1
        return (self.arena, self.lo + i0 * self.stride0, self.lo + i1 * self.stride0)


class Op:
    __slots__ = ("idx", "eng", "fn", "dma", "deps", "signal", "sem", "val", "prev_wait")


class Sched:
    COMPUTE = ("pe", "act", "dve", "pool")

    def __init__(self, nc):
        self.nc = nc
        self.ops = []
        self.iv = {}
        self.eng_obj = {"pe": nc.tensor, "act": nc.scalar, "dve": nc.vector, "pool": nc.gpsimd, "sp": nc.sync}

    def op(self, eng, fn, r=(), w=(), dma=False, mode="full"):
        o = Op()
        o.idx = len(self.ops)
        o.eng, o.fn, o.dma = eng, fn, dma
        o.signal = False
        o.deps = []
        if mode == "none":
            self.ops.append(o)
            return o
        deps = set()
        if mode == "full":
            for (ar, lo, hi) in r:
                for e in self.iv.setdefault(ar, []):
                    if e[0] < hi and lo < e[1] and e[2] is not None:
                        deps.add(e[2])
            for (ar, lo, hi) in w:
                for e in self.iv.setdefault(ar, []):
                    if e[0] < hi and lo < e[1]:
                        if e[2] is not None:
                            deps.add(e[2])
                        deps.update(e[3].values())
                        deps.update(e[4])
        deps.discard(o.idx)
        for (ar, lo, hi) in list(r) + list(w):
            lst = self.iv.setdefault(ar, [])
            new = []
            for e in lst:
                if e[0] < hi and lo < e[1] and not (lo <= e[0] and e[1] <= hi):
                    pts = sorted(set([e[0], e[1]] + [p for p in (lo, hi) if e[0] < p < e[1]]))
                    for a_, b_ in zip(pts[:-1], pts[1:]):
                        new.append([a_, b_, e[2], dict(e[3]), list(e[4])])
                else:
                    new.append(e)
            self.iv[ar] = new
        for (ar, lo, hi) in r:
            lst = self.iv[ar]
            inside = sorted([e for e in lst if lo <= e[0] and e[1] <= hi], key=lambda e: e[0])
            cur = lo
            gaps = []
            for e in inside:
                if e[0] > cur:
                    gaps.append([cur, e[0], None, {}, []])
                cur = max(cur, e[1])
                if dma:
                    e[4].append(o.idx)
                else:
                    e[3][eng] = o.idx
            if cur < hi:
                gaps.append([cur, hi, None, {}, []])
            for g_ in gaps:
                if dma:
                    g_[4].append(o.idx)
                else:
                    g_[3][eng] = o.idx
                lst.append(g_)
        for (ar, lo, hi) in w:
            lst = self.iv[ar]
            lst[:] = [e for e in lst if not (lo <= e[0] and e[1] <= hi)]
            lst.append([lo, hi, o.idx, {}, []])
        red = {}
        out = []
        for d in deps:
            p = self.ops[d]
            if p.dma:
                out.append(d)
            else:
                if p.eng == "pe" and eng == "pe" and not dma:
                    continue
                if p.eng not in red or red[p.eng] < d:
                    red[p.eng] = d
        out.extend(red.values())
        o.deps = out
        for d in out:
            self.ops[d].signal = True
        self.ops.append(o)
        return o

    def emit(self, sems_compute, sems_dma):
        cnt = {e: 0 for e in self.COMPUTE}
        dcnt = {q: 0 for q in sems_dma}
        for o in self.ops:
            if o.dma:
                q = o.eng
                n = dcnt[q]
                ns = len(sems_dma[q])
                o.sem = sems_dma[q][n % ns]
                o.val = 16 * (n // ns + 1)
                o.prev_wait = 16 * (n // ns)
                dcnt[q] = n + 1
            else:
                o.prev_wait = 0
                if o.signal:
                    cnt[o.eng] += 1
                    o.sem = sems_compute[o.eng]
                    o.val = cnt[o.eng]
        known = {}
        nwait = 0
        for o in self.ops:
            eo = self.eng_obj[o.eng]
            kn = known.setdefault(o.eng, {})
            waits = []
            for d in o.deps:
                p = self.ops[d]
                waits.append((p.sem, p.val))
            if o.dma and o.prev_wait > 0:
                waits.append((o.sem, o.prev_wait))
            for (sem, val) in waits:
                key = id(sem)
                if kn.get(key, 0) >= val:
                    continue
                kn[key] = val
                eo.wait_ge(sem, val)
                nwait += 1
            inst = o.fn()
            if o.dma:
                inst.then_inc(o.sem, 16)
            elif o.signal:
                inst.then_inc(o.sem, 1)
        for q, n in dcnt.items():
            eo = self.eng_obj[q]
            ns = len(sems_dma[q])
            for i in range(min(n, ns)):
                last_n = n - 1 - ((n - 1 - i) % ns)
                eo.wait_ge(sems_dma[q][i], 16 * (last_n // ns + 1))
        return nwait


def build_program(debug=None):
    nc = bass.Bass("TRN2", target_bir_lowering=False)
    S = Sched(nc)

    def din(name, shape, dt=F32):
        return nc.dram_tensor(name, list(shape), dt, kind="ExternalInput").ap()

    xh = din("xh", [XH_ROWS, D])
    mem = din("mem", [2, 256, D])
    flags = din("flags", [128, 4])
    invcnt = din("invcnt", [128, 2, 2, 4, 16])
    w_res_d = din("w_res", [128, 8, C_RES])
    b_res_d = din("b_res", [128, 20])
    bv_d = din("bv_bc", [128, 256])
    b_gate_d = din("b_gate", [128, 24])
    wst_d = din("wst", [8, 128, 5120])
    w_out_d = din("w_out_l", [128, 8, D])
    w_pool_d = din("w_pool_l", [128, 4, 128])
    pscale_d = din("pscale", [128, 4])
    w_memkv_d = din("w_memkv_l", [128, 8, D])
    ln_d = din("ln_bc", [6, 128, D])
    biasT_d = din("biasT", [128, 3, 16, 128])
    sink_d = din("sink_bc", [128, 16])
    wr_d = din("wr_l", [128, 8, 36])
    w_gu_d = din("w_gu", [NE, D, D])
    w_down_d = din("w_down", [NE, 512, D])
    consts_d = din("consts", [128, 419])
    y_out = nc.dram_tensor("y", [TOK, D], F32, kind="ExternalOutput").ap()

    x1_scr = nc.dram_tensor("x1_scr", [TOK, D], F32).ap()
    xs_scr = nc.dram_tensor("xs_scr", [NE * CAP + 128, D], BF16).ap()
    ys_scr = nc.dram_tensor("ys_scr", [NE * CAP + 128, D], F32).ap()
    wst_scr = nc.dram_tensor("wst_scr", [8, 128, 5120], BF16).ap()
    wres_scr = nc.dram_tensor("wres_scr", [128, 20480], BF16).ap()

    AW = 53100
    arena = nc.alloc_sbuf_tensor("arena", [128, AW], F32)

    class Alloc:
        def __init__(self):
            self.off = 0

        def buf(self, name, shape, dt):
            n = 1
            for s in shape:
                n *= s
            nb = n * DSIZE[dt]
            nb4 = (nb + 31) // 32 * 32
            lo = self.off
            assert lo + nb4 <= AW * 4, (name, lo, nb4)
            self.off += nb4
            ap = arena[:, lo // 4:(lo + nb4) // 4]
            if dt != F32:
                ap = ap.bitcast(dt)
            ap = ap[:, 0:n]
            if len(shape) == 2:
                ap = ap.rearrange("p (a b) -> p a b", b=shape[1])
            elif len(shape) == 3:
                ap = ap.rearrange("p (a b c) -> p a b c", b=shape[1], c=shape[2])
            return Buf(name, "arena", lo, shape, dt, ap)

    A = Alloc()

    psum_t = [nc.alloc_psum_tensor(f"ps{i}", [128, 512], F32) for i in range(8)]
    pstate = {"i": 0}

    class PS:
        def __init__(self, i):
            self.i = i
            self.f = psum_t[i][:, :]
            self.b = psum_t[i][:, :].bitcast(BF16)
            self.acc = (f"psum{i}", 0, 2048)

    def psum():
        i = pstate["i"]
        pstate["i"] = (i + 1) % 8
        return PS(i)

    cst = A.buf("cst", [419], F32)
    ident_f = cst.ap[:, 0:128]
    triU = cst.ap[:, 128:256]
    ones_f = cst.ap[:, 256:384]
    ebase = cst.ap[:, 384:416]
    c_mhalf = cst.ap[:, 416:417]
    trashcol = cst.ap[:, 418:419]
    c_eps = cst.ap[:, 417:418]
    ident_b = A.buf("ident_b", [128], BF16)
    flg = A.buf("flg", [4], F32)
    icn = A.buf("icn", [2 * 2 * 4, 16], F32)
    b_res = A.buf("b_res", [20], F32)
    b_gate = A.buf("b_gate", [24], F32)
    bv = A.buf("bv", [256], F32)
    pscale = A.buf("pscale", [4], F32)
    esink = A.buf("esink", [16], F32)
    wr = A.buf("wr", [8, 36], F32)
    carry = A.buf("carry", [32], F32)
    bq8 = A.buf("bq8", [8], F32)
    lg_all = A.buf("lg_all", [4, 36], F32)
    idx_all = A.buf("idx_all", [NBLK, 2], I32)
    wts_all = A.buf("wts_all", [NBLK, 2], F32)
    kmT = [A.buf(f"kmT{s}", [4, 256], BF16) for s in range(2)]
    vmaug = [A.buf(f"vmaug{s}", [2, 4, 129], BF16) for s in range(2)]
    rsmall = A.buf("rsmall", [32, 36], F32)
    mark_phase = A.off

    ln_g_in = A.buf("ln_g_in", [D], F32)
    ln_b_in = A.buf("ln_b_in", [D], F32)
    ln_g1 = A.buf("ln_g1", [D], F32)
    ln_b1 = A.buf("ln_b1", [D], F32)
    biasT = A.buf("biasT", [3, 16, 128], BF16)
    w_out = A.buf("w_out", [8, D], BF16)
    w_pool = A.buf("w_pool", [4, 128], BF16)
    wstb = [A.buf(f"wst{i}", [40, 128], BF16) for i in range(2)]
    mark_act = A.off

    def ld(eng, dst_ap, src_ap, w, r=()):
        return S.op(eng, lambda: S.eng_obj[eng].dma_start(out=dst_ap, in_=src_ap), r=r, w=w, dma=True)

    ld("sp", cst.ap, consts_d[:, :], [cst.a()])
    ld("sp", flg.ap, flags[:, :], [flg.a()])
    ld("sp", icn.ap, invcnt.rearrange("p s e g t -> p (s e g) t"), [icn.a()])
    ld("sp", b_res.ap, b_res_d[:, :], [b_res.a()])
    ld("sp", b_gate.ap, b_gate_d[:, :], [b_gate.a()])
    ld("sp", bv.ap, bv_d[:, :], [bv.a()])
    ld("sp", pscale.ap, pscale_d[:, :], [pscale.a()])
    ld("sp", esink.ap, sink_d[:, :], [esink.a()])
    ld("sp", wr.ap, wr_d[:, :, :], [wr.a()])
    ld("sp", ln_g_in.ap, ln_d[0], [ln_g_in.a()])
    ld("sp", ln_b_in.ap, ln_d[1], [ln_b_in.a()])
    ld("sp", ln_g1.ap, ln_d[2], [ln_g1.a()])
    ld("sp", ln_b1.ap, ln_d[3], [ln_b1.a()])
    for r_ in range(3):
        ld("pool", biasT.ap[:, r_], biasT_d[:, r_], [biasT.a(r_)])
    ld("pool", w_pool.ap, w_pool_d[:, :, :], [w_pool.a()])
    S.op("act", lambda: nc.scalar.activation(out=esink.ap, in_=esink.ap, func=AF.Exp), r=[esink.a()], w=[esink.a()])
    S.op("act", lambda: nc.scalar.copy(out=ident_b.ap, in_=ident_f), r=[cst.a()], w=[ident_b.a()])
    S.op("pool", lambda: nc.gpsimd.memset(carry.ap, 0.0), w=[carry.a()])
    S.op("dve", lambda: nc.vector.tensor_scalar(out=bq8.ap, in0=b_res.ap[:, 4:12], scalar1=0.125, scalar2=None, op0=ALU.mult), r=[b_res.a()], w=[bq8.a()])
    S.op("pool", lambda: nc.gpsimd.affine_select(out=biasT.ap[:, 0], in_=biasT.ap[:, 0], pattern=[[0, 16], [-1, 128]],
                                                 compare_op=ALU.is_ge, fill=NEG, base=0, channel_multiplier=1),
         r=[biasT.a(0)], w=[biasT.a(0)])
    S.op("pool", lambda: nc.gpsimd.affine_select(out=biasT.ap[:, 2], in_=biasT.ap[:, 2], pattern=[[0, 16], [1, 128]],
                                                 compare_op=ALU.is_ge, fill=NEG, base=0, channel_multiplier=-1),
         r=[biasT.a(2)], w=[biasT.a(2)])

    def mm_group(ps_ap, pairs, ps_acc, reads):
        n = len(pairs)
        last = None
        for i, (l, r_) in enumerate(pairs):
            def f(l=l, r_=r_, i=i):
                return nc.tensor.matmul(ps_ap, l, r_, start=(i == 0), stop=(i == n - 1))
            if i == 0:
                mode = "full"
            elif i == n - 1:
                mode = "reg"
            else:
                mode = "none"
            last = S.op("pe", f, r=reads, w=[ps_acc], mode=mode)
        return last

    def ln_stats(src_ap, src_acc, stat):
        st6 = stat.ap[:, 0:12].rearrange("p (a b) -> p a b", b=6)
        for h in range(2):
            S.op("dve", lambda h=h: nc.vector.bn_stats(out=st6[:, h, :], in_=src_ap[:, h * 512:(h + 1) * 512]),
                 r=[src_acc], w=[stat.a(0)])
        S.op("dve", lambda: nc.vector.bn_aggr(out=stat.ap[:, 12:14], in_=stat.ap[:, 0:12]), r=[stat.a(0)], w=[stat.a(0)])
        S.op("act", lambda: nc.scalar.activation(out=stat.ap[:, 14:15], in_=stat.ap[:, 13:14], func=AF.Ln, bias=c_eps),
             r=[stat.a(0), cst.a()], w=[stat.a(0)])
        S.op("act", lambda: nc.scalar.activation(out=stat.ap[:, 15:16], in_=stat.ap[:, 14:15], func=AF.Exp, scale=-0.5),
             r=[stat.a(0)], w=[stat.a(0)])

    def ln_apply(src_ap, src_acc, dst_ap, dst_acc, g_buf, b_buf, stat):
        S.op("dve", lambda: nc.vector.scalar_tensor_tensor(out=dst_ap, in0=src_ap, scalar=stat.ap[:, 12:13], in1=g_buf.ap, op0=ALU.subtract, op1=ALU.mult),
             r=[src_acc, stat.a(0), g_buf.a()], w=[dst_acc])
        S.op("dve", lambda: nc.vector.scalar_tensor_tensor(out=dst_ap, in0=dst_ap, scalar=stat.ap[:, 15:16], in1=b_buf.ap, op0=ALU.mult, op1=ALU.add),
             r=[dst_acc, stat.a(0), b_buf.a()], w=[dst_acc])

    def ln_apply2(src_ap, src_acc, mid_ap, mid_acc, dst_ap, dst_acc, g_buf, b_buf, stat):
        S.op("dve", lambda: nc.vector.scalar_tensor_tensor(out=mid_ap, in0=src_ap, scalar=stat.ap[:, 12:13], in1=g_buf.ap, op0=ALU.subtract, op1=ALU.mult),
             r=[src_acc, stat.a(0), g_buf.a()], w=[mid_acc])
        S.op("dve", lambda: nc.vector.scalar_tensor_tensor(out=dst_ap, in0=mid_ap, scalar=stat.ap[:, 15:16], in1=b_buf.ap, op0=ALU.mult, op1=ALU.add),
             r=[mid_acc, stat.a(0), b_buf.a()], w=[dst_acc])

    def layer_norm_tok(src_ap, src_acc, dst_ap, dst_acc, g_buf, b_buf, tmp, stat):
        ln_stats(src_ap, src_acc, stat)
        ln_apply(src_ap, src_acc, dst_ap, dst_acc, g_buf, b_buf, stat)

    A.off = mark_act
    xin = [A.buf(f"xin{i}", [D], F32) for i in range(2)]
    lntmp = None
    lnstat = [A.buf(f"lnstat{i}", [16], F32) for i in range(2)]
    xn_c = A.buf("xn_c", [4, D], F32)
    xnb = [A.buf(f"xnb{i}", [D], BF16) for i in range(2)]
    xnT = A.buf("xnT", [8, 768], BF16)
    mixedT = A.buf("mixedT", [4, 512], BF16)
    oT = A.buf("oT", [8, 512], BF16)
    omT = A.buf("omT", [4, 512], BF16)
    xin.append(Buf("xin2", "arena", mixedT.lo, [D], F32, arena[:, mixedT.lo // 4:mixedT.lo // 4 + D]))
    xin.append(Buf("xin3", "arena", omT.lo, [D], F32, arena[:, omT.lo // 4:omT.lo // 4 + D]))
    mark_stage = A.off
    w_u = A.buf("w_u", [8, 512], BF16)
    w_q = A.buf("w_q", [8, 1024], BF16)
    w_k = A.buf("w_k", [8, 256], BF16)
    w_v = A.buf("w_v", [8, 256], BF16)
    w_qm = A.buf("w_qm", [8, 512], BF16)
    mark_bcd = A.off
    assert mark_bcd - mark_stage == 40960
    wres_flat = Buf("wres_flat", "arena", mark_stage, [20480], BF16, arena[:, mark_stage // 4:mark_bcd // 4].bitcast(BF16))
    uT = A.buf("uT", [4, 544], F32)
    ptA = A.buf("ptA", [544], F32)
    ptB = A.buf("ptB", [544], F32)
    pooledT = A.buf("pooledT", [4, 512], BF16)
    endB = A.off
    A.off = mark_bcd
    kT = A.buf("kT", [2, 768], BF16)
    vaug = A.buf("vaug", [6, 4, 65], BF16)
    PT = [A.buf(f"PT{i}", [512], BF16) for i in range(6)]
    o_sb = [A.buf(f"o_sb{i}", [D], BF16) for i in range(2)]
    den = [A.buf(f"den{i}", [8], F32) for i in range(2)]
    assert A.off <= endB
    A.off = mark_bcd + 8704 + 9600
    qT = A.buf("qT", [8, 512], BF16)
    endC = A.off
    A.off = mark_bcd
    qmT = A.buf("qmT", [4, 512], BF16)
    PmT = A.buf("PmT", [8, 512], BF16)
    om = [A.buf(f"om{i}", [512], BF16) for i in range(2)]
    dend = [A.buf(f"dend{i}", [4], F32) for i in range(2)]
    endD = A.off
    A.off = mark_stage
    mergedT = A.buf("mergedT", [8, 512], BF16)
    gsig = [A.buf(f"gsig{i}", [512], F32) for i in range(3)]
    macc = [A.buf(f"macc{i}", [512], F32) for i in range(2)]
    x1 = [A.buf(f"x1_{i}", [D], F32) for i in range(4)]
    x1T = [A.buf(f"x1T{i}", [8, 128], F32) for i in range(2)]
    lnstatF = [A.buf(f"lnstatF{i}", [16], F32) for i in range(4)]
    assert A.off <= mark_bcd + 8704
    x1b = [Buf(f"x1b{i}", "arena", oT.lo + i * 2048, [D], BF16, arena[:, (oT.lo + i * 2048) // 4:(oT.lo + (i + 1) * 2048) // 4].bitcast(BF16)) for i in range(4)]
    A.off = mark_bcd + 8704
    rbig = A.buf("rbig", [2400], F32)
    endE = A.off
    print("[kernel] sbuf marks", mark_phase, mark_act, mark_stage, endB, endC, endD, endE, AW * 4)
    assert max(endB, endC, endD, endE) <= AW * 4
    S.op("pool", lambda: nc.gpsimd.memset(xin[0].ap, 0.0), w=[xin[0].a()])
    S.op("sp", lambda: nc.sync.dma_start(out=ys_scr[NE * CAP:NE * CAP + 128, :], in_=xin[0].ap), r=[xin[0].a()], w=[("ys_scr", NE, NE + 1)], dma=True)
    col = 0
    for wb_, ncol in ((w_u, 512), (w_q, 1024), (w_k, 256), (w_v, 256), (w_qm, 512)):
        ld("pool", wb_.ap, w_res_d[:, :, col:col + ncol], [wb_.a()])
        col += ncol
    S.op("sp", lambda: nc.sync.dma_start(out=wres_scr[:, :], in_=wres_flat.ap), r=[wres_flat.a()], w=[("wres_scr", 0, 1)], dma=True)

    def stage_wst():
        ld("pool", w_out.ap, w_out_d[:, :, :], [w_out.a()])
        for c in range(8):
            wb = wstb[c % 2]
            for h in range(4):
                ld("pool", wb.ap[:, 10 * h:10 * h + 10, :], wst_d[c, :, 1280 * h:1280 * (h + 1)].rearrange("p (a b) -> p a b", b=128), [wb.a(10 * h, 10 * h + 10)])
            S.op("sp", lambda c=c, wb=wb: nc.sync.dma_start(out=wst_scr[c].rearrange("p (a b) -> p a b", b=128), in_=wb.ap),
                 r=[wb.a()], w=[("wst_scr", c, c + 1)], dma=True)

    A.off = mark_bcd
    wkv = A.buf("wkv", [8, D], BF16)
    memx = A.buf("memx", [D], F32)
    memb = A.buf("memb", [D], BF16)
    memT = A.buf("memT", [8, 256], BF16)
    ld("pool", wkv.ap, w_memkv_d[:, :, :], [wkv.a()])
    for s in range(2):
        for mb in range(2):
            ld("sp", memx.ap, mem[s, mb * 128:(mb + 1) * 128, :], [memx.a()])
            S.op("act", lambda: nc.scalar.copy(out=memb.ap, in_=memx.ap), r=[memx.a()], w=[memb.a()])
            ps = psum()
            for k in range(8):
                S.op("pe", lambda k=k, ps=ps: nc.tensor.transpose(ps.b[:, k * 128:(k + 1) * 128], memb.ap[:, k * 128:(k + 1) * 128], ident_b.ap),
                     r=[memb.a(), ident_b.a()], w=[ps.acc])
            S.op("dve", lambda ps=ps, mb=mb: nc.vector.tensor_copy(out=memT.ap[:, :, mb * 128:(mb + 1) * 128],
                                                                     in_=ps.b[:, :].rearrange("p (a b) -> p a b", b=128)),
                 r=[ps.acc], w=[memT.a()])
        for hm in range(4):
            ps = psum()
            mm_group(ps.f[:, 0:256], [(wkv.ap[:, k, hm * 128:(hm + 1) * 128], memT.ap[:, k, :]) for k in range(8)], ps.acc, [wkv.a(), memT.a()])
            S.op("act", lambda ps=ps, hm=hm, s=s: nc.scalar.copy(out=kmT[s].ap[:, hm, :], in_=ps.f[:, 0:256]), r=[ps.acc], w=[kmT[s].a(hm)])
        S.op("pool", lambda s=s: nc.gpsimd.memset(vmaug[s].ap[:, :, :, 128:129], 1.0), w=[vmaug[s].a()])
        for mc in range(2):
            ps = psum()
            mm_group(ps.f[:, :], [(memT.ap[:, k, mc * 128:(mc + 1) * 128], wkv.ap[:, k, 512:1024]) for k in range(8)], ps.acc, [wkv.a(), memT.a()])
            S.op("dve", lambda ps=ps, mc=mc, s=s: nc.vector.tensor_copy(out=vmaug[s].ap[:, mc, :, 0:128], in_=ps.f[:, :].rearrange("p (a b) -> p a b", b=128)),
                 r=[ps.acc], w=[vmaug[s].a(mc)])

    blk_counter = {"n": 0}
    pending = {}

    def supertile(ti):
        seg, st, nst = TILES[ti]
        row0 = SEGS[seg][0] + st * NT
        first, last = (st == 0), (st == nst - 1)
        if not (seg == 0 and st == 0):
            S.op("act", lambda: nc.scalar.dma_start(out=wres_flat.ap, in_=wres_scr[:, :]), r=[("wres_scr", 0, 1)], w=[wres_flat.a()], dma=True)
        def a1_load_stats(bw):
            xi = xin[bw % 4]
            if not (ti > 0 and bw < 4):
                ld("sp", xi.ap, xh[row0 + bw * 128: row0 + (bw + 1) * 128, :], [xi.a()])
            ln_stats(xi.ap, xi.a(), lnstat[bw % 2])

        a1_load_stats(0)
        pend_evac = None
        for bw in range(6):
            xi = xin[bw % 4]
            xb = xnb[bw % 2]
            center = 1 <= bw <= 4
            if bw + 1 < 6:
                a1_load_stats(bw + 1)
            if center:
                dst_ap, dst_acc = xn_c.ap[:, bw - 1, :], xn_c.a(bw - 1)
                ln_apply(xi.ap, xi.a(), dst_ap, dst_acc, ln_g_in, ln_b_in, lnstat[bw % 2])
                S.op("act", lambda dst_ap=dst_ap, xb=xb: nc.scalar.copy(out=xb.ap, in_=dst_ap), r=[dst_acc], w=[xb.a()])
            else:
                ln_apply2(xi.ap, xi.a(), xi.ap, xi.a(), xb.ap, xb.a(), ln_g_in, ln_b_in, lnstat[bw % 2])
            ps = psum()
            for k in range(8):
                S.op("pe", lambda k=k, ps=ps, xb=xb: nc.tensor.transpose(ps.b[:, k * 128:(k + 1) * 128], xb.ap[:, k * 128:(k + 1) * 128], ident_b.ap),
                     r=[xb.a(), ident_b.a()], w=[ps.acc])
            if pend_evac is not None:
                pend_evac()

            def mk(ps=ps, bw=bw):
                def f():
                    S.op("act", lambda: nc.scalar.copy(out=xnT.ap[:, :, bw * 128:(bw + 1) * 128], in_=ps.b[:, :].rearrange("p (a b) -> p a b", b=128)),
                         r=[ps.acc], w=[xnT.a()])
                return f
            pend_evac = mk()
        pend_evac()

        if pending.get("route") is not None:
            pending.pop("route")()
        if ti == 0:
            stage_wst()
        if ti + 1 < len(TILES):
            nseg, nst_, _ = TILES[ti + 1]
            nrow0 = SEGS[nseg][0] + nst_ * NT
            for bw in range(2):
                ld("sp", xin[bw].ap, xh[nrow0 + bw * 128: nrow0 + (bw + 1) * 128, :], [xin[bw].a()])

        def proj_fm(wbuf, nch, t0, t1, evac):
            for m in range(nch):
                tt = t0
                while tt < t1:
                    n = min(512, t1 - tt)
                    ps = psum()
                    mm_group(ps.f[:, 0:n], [(wbuf.ap[:, k, m * 128:(m + 1) * 128], xnT.ap[:, k, tt:tt + n]) for k in range(8)],
                             ps.acc, [wbuf.a(), xnT.a()])
                    evac(m, tt - t0, n, ps)
                    tt += n

        def ev_u(m, o, n, ps):
            S.op("act", lambda: nc.scalar.activation(out=uT.ap[:, m, o:o + n], in_=ps.f[:, 0:n], func=AF.Identity, bias=b_res.ap[:, m:m + 1]),
                 r=[ps.acc, b_res.a()], w=[uT.a(m)])
        proj_fm(w_u, 4, 112, 656, ev_u)
        def ev_q(m, o, n, ps):
            S.op("act", lambda: nc.scalar.activation(out=qT.ap[:, m, o:o + n], in_=ps.f[:, 0:n], func=AF.Identity, bias=bq8.ap[:, m:m + 1], scale=0.125),
                 r=[ps.acc, bq8.a()], w=[qT.a(m)])
        proj_fm(w_q, 8, 128, 640, ev_q)

        if first:
            S.op("dve", lambda: nc.vector.tensor_scalar(out=uT.ap[:, :, 0:16], in0=uT.ap[:, :, 0:16], scalar1=flg.ap[:, 2 * seg:2 * seg + 1], scalar2=None, op0=ALU.mult),
                 r=[uT.a(), flg.a()], w=[uT.a()])
        if last:
            S.op("dve", lambda: nc.vector.tensor_scalar(out=uT.ap[:, :, 528:544], in0=uT.ap[:, :, 528:544], scalar1=flg.ap[:, 2 * seg + 1:2 * seg + 2], scalar2=None, op0=ALU.mult),
                 r=[uT.a(), flg.a()], w=[uT.a()])
        for g in range(4):
            w_ = 2 << g
            h_ = w_ // 2
            u = uT.ap[:, g, :]
            src, src_acc = u, uT.a(g)
            m = 1
            bufs = [ptA, ptB]
            bi = 0
            while m < h_:
                dstb = bufs[bi]
                L = 544 - m
                S.op("dve", lambda src=src, dstb=dstb, m=m, L=L: nc.vector.tensor_tensor(out=dstb.ap[:, 0:L - m], in0=src[:, 0:L - m], in1=src[:, m:L], op=ALU.add),
                     r=[src_acc], w=[dstb.a()])
                src, src_acc = dstb.ap, dstb.a()
                m *= 2
                bi ^= 1
            dstb = bufs[bi]
            S.op("dve", lambda src=src, dstb=dstb, h_=h_: nc.vector.tensor_tensor(out=dstb.ap[:, 16:528], in0=src[:, 16 - h_:528 - h_], in1=src[:, 16:528], op=ALU.add),
                 r=[src_acc], w=[dstb.a()])
            S.op("dve", lambda dstb=dstb, g=g, w_=w_, u=u: nc.vector.scalar_tensor_tensor(out=pooledT.ap[:, g, :], in0=dstb.ap[:, 16:528], scalar=1.0 / w_, in1=u[:, 16:528],
                                                                                             op0=ALU.mult, op1=ALU.subtract),
                 r=[dstb.a(), uT.a(g)], w=[pooledT.a(g)])
            for (cond, e, c0) in ((first, 0, 0), (last, 1, 496)):
                if not cond:
                    continue
                ic = icn.ap[:, (seg * 2 + e) * 4 + g, :]
                S.op("dve", lambda dstb=dstb, ic=ic, c0=c0: nc.vector.tensor_tensor(out=dstb.ap[:, 16 + c0:32 + c0], in0=dstb.ap[:, 16 + c0:32 + c0], in1=ic, op=ALU.mult),
                     r=[dstb.a(), icn.a()], w=[dstb.a()])
                S.op("dve", lambda dstb=dstb, c0=c0, g=g, u=u: nc.vector.tensor_tensor(out=pooledT.ap[:, g, c0:c0 + 16], in0=dstb.ap[:, 16 + c0:32 + c0], in1=u[:, 16 + c0:32 + c0], op=ALU.subtract),
                     r=[dstb.a(), uT.a(g)], w=[pooledT.a(g)])
            ps = psum()
            mm_group(ps.f[:, :], [(w_pool.ap[:, g, :], pooledT.ap[:, g, :])], ps.acc, [w_pool.a(), pooledT.a(g)])
            S.op("act", lambda ps=ps, g=g: nc.scalar.activation(out=mixedT.ap[:, g, :], in_=ps.f[:, :], func=AF.Copy, scale=pscale.ap[:, g:g + 1]),
                 r=[ps.acc, pscale.a()], w=[mixedT.a(g)])

        def ev_k(m, o, n, ps):
            S.op("dve", lambda: nc.vector.tensor_scalar(out=kT.ap[:, m, o:o + n], in0=ps.f[:, 0:n], scalar1=b_res.ap[:, 12 + m:13 + m], scalar2=None, op0=ALU.add),
                 r=[ps.acc, b_res.a()], w=[kT.a(m)])
        proj_fm(w_k, 2, 0, 768, ev_k)
        S.op("act", lambda: nc.scalar.copy(out=vaug.ap[:, :, :, 64:65], in_=ones_f[:, 0:24].rearrange("p (a b c) -> p a b c", a=6, b=4)),
             r=[cst.a()], w=[vaug.a()])
        for bw in range(6):
            ps = psum()
            mm_group(ps.f[:, 0:256], [(xnT.ap[:, k, bw * 128:(bw + 1) * 128], w_v.ap[:, k, :]) for k in range(8)], ps.acc, [w_v.a(), xnT.a()])
            S.op("dve", lambda ps=ps, bw=bw: nc.vector.tensor_tensor(out=vaug.ap[:, bw, :, 0:64], in0=ps.f[:, 0:256].rearrange("p (a b) -> p a b", b=64),
                                                                      in1=bv.ap.rearrange("p (a b) -> p a b", b=64), op=ALU.add),
                 r=[ps.acc, bv.a()], w=[vaug.a(bw)])
        if first:
            S.op("dve", lambda: nc.vector.tensor_scalar(out=vaug.ap[:, 0], in0=vaug.ap[:, 0], scalar1=flg.ap[:, 2 * seg:2 * seg + 1], scalar2=None, op0=ALU.mult),
                 r=[vaug.a(0), flg.a()], w=[vaug.a(0)])
        if last:
            S.op("dve", lambda: nc.vector.tensor_scalar(out=vaug.ap[:, 5], in0=vaug.ap[:, 5], scalar1=flg.ap[:, 2 * seg + 1:2 * seg + 2], scalar2=None, op0=ALU.mult),
                 r=[vaug.a(5), flg.a()], w=[vaug.a(5)])

        units = [(qb, g) for qb in range(4) for g in range(4)]
        cstate = {}

        def c_scores(ui):
            qb, g = units[ui]
            pc = 64 * (g % 2)
            kc = g // 2
            pts = []
            for r_ in range(3):
                bwk = qb + r_
                ps = psum()
                psv = ps.f[:, :].rearrange("p (a b) -> p a b", b=128)
                S.op("pe", lambda ps=ps, psv=psv, bwk=bwk, pc=pc, kc=kc, qb=qb: nc.tensor.matmul(
                    psv, kT.ap[pc:pc + 64, kc, bwk * 128:(bwk + 1) * 128], qT.ap[pc:pc + 64, kc * 4:kc * 4 + 4, qb * 128:(qb + 1) * 128], start=True, stop=False),
                    r=[kT.a(kc), qT.a(kc * 4, kc * 4 + 4)], w=[ps.acc])
                S.op("pe", lambda ps=ps, psv=psv, r_=r_, g=g: nc.tensor.matmul(psv, ident_b.ap, biasT.ap[:, r_, 4 * g:4 * g + 4, :], start=False, stop=True),
                     r=[ident_b.a(), biasT.a(r_)], w=[ps.acc])
                pt = PT[(ui * 3 + r_) % 6]
                S.op("act", lambda ps=ps, pt=pt: nc.scalar.activation(out=pt.ap, in_=ps.f[:, :], func=AF.Exp), r=[ps.acc], w=[pt.a()])
                pts.append(pt)
            cstate[ui] = pts

        def c_out(ui):
            qb, g = units[ui]
            pts = cstate.pop(ui)
            osb = o_sb[qb % 2]
            dn = den[ui % 2]
            po = psum()
            for j in range(4):
                mm_group(po.f[:, j * 65:(j + 1) * 65],
                         [(pts[r_].ap[:, j * 128:(j + 1) * 128], vaug.ap[:, qb + r_, g, :]) for r_ in range(3)],
                         po.acc, [pts[0].a(), pts[1].a(), pts[2].a(), vaug.a(qb, qb + 3)])
            pov = po.f[:, 0:260].rearrange("p (a b) -> p a b", b=65)
            S.op("dve", lambda: nc.vector.tensor_tensor(out=dn.ap[:, 0:4], in0=pov[:, :, 64], in1=esink.ap[:, 4 * g:4 * g + 4], op=ALU.add),
                 r=[po.acc, esink.a()], w=[dn.a()])
            S.op("dve", lambda: nc.vector.reciprocal(out=dn.ap[:, 4:8], in_=dn.ap[:, 0:4]), r=[dn.a()], w=[dn.a()])
            S.op("dve", lambda: nc.vector.tensor_tensor(
                out=osb.ap[:, g * 256:(g + 1) * 256].rearrange("p (a b) -> p a b", b=64), in0=pov[:, :, 0:64],
                in1=dn.ap[:, 4:8].unsqueeze(2).to_broadcast([128, 4, 64]), op=ALU.mult),
                r=[po.acc, dn.a()], w=[osb.a(g * 256 // 1024, 1)] if False else [osb.a()])
            if g == 3:
                ps = psum()
                for k in range(8):
                    S.op("pe", lambda k=k, ps=ps: nc.tensor.transpose(ps.b[:, k * 128:(k + 1) * 128], osb.ap[:, k * 128:(k + 1) * 128], ident_b.ap),
                         r=[osb.a(), ident_b.a()], w=[ps.acc])
                S.op("act", lambda ps=ps: nc.scalar.copy(out=oT.ap[:, :, qb * 128:(qb + 1) * 128], in_=ps.b[:, :].rearrange("p (a b) -> p a b", b=128)),
                     r=[ps.acc], w=[oT.a()])

        c_scores(0)
        for ui in range(16):
            if ui + 1 < 16:
                c_scores(ui + 1)
            c_out(ui)

        def ev_qm(m, o, n, ps):
            S.op("act", lambda: nc.scalar.activation(out=qmT.ap[:, m, o:o + n], in_=ps.f[:, 0:n], func=AF.Identity, bias=b_res.ap[:, 16 + m:17 + m]),
                 r=[ps.acc, b_res.a()], w=[qmT.a(m)])
        proj_fm(w_qm, 4, 128, 640, ev_qm)
        for hm in range(4):
            for mc in range(2):
                ps = psum()
                mm_group(ps.f[:, :], [(kmT[seg].ap[:, hm, mc * 128:(mc + 1) * 128], qmT.ap[:, hm, :])], ps.acc, [kmT[seg].a(hm), qmT.a(hm)])
                S.op("act", lambda ps=ps, hm=hm, mc=mc: nc.scalar.activation(out=PmT.ap[:, hm * 2 + mc, :], in_=ps.f[:, :], func=AF.Exp, scale=128.0 ** -0.5),
                     r=[ps.acc], w=[PmT.a(hm * 2 + mc)])
        for qb in range(4):
            omb = om[qb % 2]
            dd = dend[qb % 2]
            for hh in range(2):
                po = psum()
                for j in range(2):
                    hm = hh * 2 + j
                    mm_group(po.f[:, j * 129:(j + 1) * 129],
                             [(PmT.ap[:, hm * 2 + mc, qb * 128:(qb + 1) * 128], vmaug[seg].ap[:, mc, hm, :]) for mc in range(2)],
                             po.acc, [PmT.a(hm * 2, hm * 2 + 2), vmaug[seg].a()])
                pov = po.f[:, 0:258].rearrange("p (a b) -> p a b", b=129)
                S.op("dve", lambda pov=pov, dd=dd: nc.vector.reciprocal(out=dd.ap[:, 0:2], in_=pov[:, :, 128]), r=[po.acc], w=[dd.a()])
                S.op("dve", lambda pov=pov, dd=dd, hh=hh, omb=omb: nc.vector.tensor_tensor(
                    out=omb.ap[:, hh * 256:(hh + 1) * 256].rearrange("p (a b) -> p a b", b=128), in0=pov[:, :, 0:128],
                    in1=dd.ap[:, 0:2].unsqueeze(2).to_broadcast([128, 2, 128]), op=ALU.mult),
                    r=[po.acc, dd.a()], w=[omb.a()])
            ps = psum()
            for k in range(4):
                S.op("pe", lambda k=k, ps=ps, omb=omb: nc.tensor.transpose(ps.b[:, k * 128:(k + 1) * 128], omb.ap[:, k * 128:(k + 1) * 128], ident_b.ap),
                     r=[omb.a(), ident_b.a()], w=[ps.acc])
            S.op("act", lambda ps=ps, qb=qb: nc.scalar.copy(out=omT.ap[:, :, qb * 128:(qb + 1) * 128], in_=ps.b[:, 0:512].rearrange("p (a b) -> p a b", b=128)),
                 r=[ps.acc], w=[omT.a()])

        gi = 0
        for c in range(8):
            wb = wstb[c % 2]
            S.op("sp", lambda c=c, wb=wb: nc.sync.dma_start(out=wb.ap, in_=wst_scr[c].rearrange("p (a b) -> p a b", b=128)),
                 r=[("wst_scr", c, c + 1)], w=[wb.a()], dma=True)
            mc_ = macc[c % 2]
            branches = [(24, 4, mixedT), (28, 8, oT), (36, 4, omT)]
            for r_ in range(3):
                pg = psum()
                mm_group(pg.f[:, :], [(wb.ap[:, r_ * 8 + k, :], xnT.ap[:, k, 128:640]) for k in range(8)], pg.acc, [wb.a(), xnT.a()])
                gs = gsig[gi % 3]
                gi += 1
                S.op("act", lambda pg=pg, gs=gs, r_=r_, c=c: nc.scalar.activation(out=gs.ap, in_=pg.f[:, :], func=AF.Sigmoid, bias=b_gate.ap[:, r_ * 8 + c:r_ * 8 + c + 1]),
                     r=[pg.acc, b_gate.a()], w=[gs.a()])
                s0, nk, src = branches[r_]
                pb = psum()
                mm_group(pb.f[:, :], [(wb.ap[:, s0 + k, :], src.ap[:, k, :]) for k in range(nk)], pb.acc, [wb.a(), src.a()])
                if r_ == 0:
                    S.op("dve", lambda gs=gs, pb=pb, mc_=mc_: nc.vector.tensor_tensor(out=mc_.ap, in0=gs.ap, in1=pb.f[:, :], op=ALU.mult),
                         r=[gs.a(), pb.acc], w=[mc_.a()])
                else:
                    S.op("dve", lambda gs=gs, pb=pb: nc.vector.tensor_tensor(out=gs.ap, in0=gs.ap, in1=pb.f[:, :], op=ALU.mult),
                         r=[gs.a(), pb.acc], w=[gs.a()])
                    if r_ == 1:
                        S.op("dve", lambda gs=gs, mc_=mc_: nc.vector.tensor_tensor(out=mc_.ap, in0=mc_.ap, in1=gs.ap, op=ALU.add),
                             r=[gs.a(), mc_.a()], w=[mc_.a()])
                    else:
                        S.op("dve", lambda gs=gs, mc_=mc_, c=c: nc.vector.tensor_tensor(out=mergedT.ap[:, c, :], in0=mc_.ap, in1=gs.ap, op=ALU.add),
                             r=[gs.a(), mc_.a()], w=[mergedT.a(c)])

        if ti + 1 < len(TILES):
            nseg, nst_, _ = TILES[ti + 1]
            nrow0 = SEGS[nseg][0] + nst_ * NT
            for bw in (2, 3):
                ld("sp", xin[bw].ap, xh[nrow0 + bw * 128: nrow0 + (bw + 1) * 128, :], [xin[bw].a()])
        blk0 = blk_counter["n"]
        blk_counter["n"] += 4
        for qb in range(4):
            xo = x1[qb]
            for n in range(2):
                ps = psum()
                mm_group(ps.f[:, :], [(mergedT.ap[:, k, qb * 128:(qb + 1) * 128], w_out.ap[:, k, n * 512:(n + 1) * 512]) for k in range(8)],
                         ps.acc, [mergedT.a(), w_out.a()])
                S.op("dve", lambda ps=ps, n=n, xo=xo, qb=qb: nc.vector.scalar_tensor_tensor(out=xo.ap[:, n * 512:(n + 1) * 512], in0=xn_c.ap[:, qb, n * 512:(n + 1) * 512],
                                                                                           scalar=ALPHA, in1=ps.f[:, :], op0=ALU.mult, op1=ALU.add),
                     r=[ps.acc, xn_c.a(qb)], w=[xo.a()])
        for qb in range(4):
            ln_stats(x1[qb].ap, x1[qb].a(), lnstatF[qb])
        for qb in range(4):
            blk = blk0 + qb
            xo, xob = x1[qb], x1b[qb]
            ln_apply(xo.ap, xo.a(), xo.ap, xo.a(), ln_g1, ln_b1, lnstatF[qb])
            S.op("sp", lambda xo=xo, blk=blk: nc.sync.dma_start(out=x1_scr[blk * 128:(blk + 1) * 128, :], in_=xo.ap),
                 r=[xo.a()], w=[("x1_scr", blk, blk + 1)], dma=True)
            S.op("act", lambda xo=xo, xob=xob: nc.scalar.copy(out=xob.ap, in_=xo.ap), r=[xo.a()], w=[xob.a()])

        def f_transposes(qb):
            xo = x1[qb]
            xt = x1T[qb % 2]
            for hh in range(2):
                ps = psum()
                for k in range(4):
                    kk = hh * 4 + k
                    S.op("pe", lambda k=k, kk=kk, ps=ps: nc.tensor.transpose(ps.f[:, k * 128:(k + 1) * 128], xo.ap[:, kk * 128:(kk + 1) * 128], ident_f),
                         r=[xo.a(), cst.a()], w=[ps.acc])
                S.op("act", lambda ps=ps, hh=hh: nc.scalar.copy(out=xt.ap[:, hh * 4:hh * 4 + 4, :], in_=ps.f[:, :].rearrange("p (a b) -> p a b", b=128)),
                     r=[ps.acc], w=[xt.a(hh * 4, hh * 4 + 4)])

        def f_logits(qb):
            xt = x1T[qb % 2]
            ps = psum()
            mm_group(ps.f[:, 0:36], [(xt.ap[:, k, :], wr.ap[:, k, :]) for k in range(8)], ps.acc, [xt.a(), wr.a()])
            S.op("dve", lambda: nc.vector.tensor_copy(out=lg_all.ap[:, qb, :], in_=ps.f[:, 0:36]), r=[ps.acc], w=[lg_all.a(qb)])

        f_transposes(0)
        for qb in range(4):
            if qb + 1 < 4:
                f_transposes(qb + 1)
            f_logits(qb)

        def route_and_scatter():
            router4(blk0)
            for qb in range(4):
                blk = blk0 + qb
                xob = x1b[qb]
                for kk in range(2):
                    S.op("pool", lambda xob=xob, blk=blk, kk=kk: nc.gpsimd.indirect_dma_start(
                        out=xs_scr[:, :], out_offset=bass.IndirectOffsetOnAxis(ap=idx_all.ap[:, blk, kk:kk + 1].bitcast(U32), axis=0),
                        in_=xob.ap, in_offset=None),
                        r=[xob.a(), idx_all.a(blk)], w=[("xs_scr", 0, NE * CAP)], dma=True)
        if ti + 1 < len(TILES):
            pending["route"] = route_and_scatter
        else:
            route_and_scatter()

    def router4(blk0):
        rows = {}

        BIG = ("elm", "sel", "oh1", "oh2", "esh", "ee", "wraw", "wmat", "t1", "t2", "dest", "ovf", "tr", "t3", "t4")
        nbig = {"n": 0}
        nsmall = {"n": 0}

        def row(name):
            if name not in rows:
                if name in BIG:
                    rows[name] = (nbig["n"] * 128, 128)
                    nbig["n"] += 1
                    assert nbig["n"] <= 15
                else:
                    rows[name] = (15 * 128 + nsmall["n"] * 32, 32)
                    nsmall["n"] += 1
                    assert nsmall["n"] <= 15
            off, n = rows[name]
            return rbig.ap[:, off:off + n], ("arena", rbig.lo + off * 4, rbig.lo + (off + n) * 4)

        def v3(ap, n):
            return ap[:, 0:4 * n].rearrange("p (a b) -> p a b", b=n)

        def dv(fn, r, w):
            S.op("dve", fn, r=r, w=w)

        def bc(ap1, n):
            return ap1.to_broadcast([128, 4, n])

        lg = lg_all.ap
        lg_a = lg_all.a()
        gl = lg[:, :, 0:4]
        r_, gmax_a = row("gmax"); gmax = r_[:, 0:4]
        dv(lambda: nc.vector.tensor_reduce(out=gmax, in_=gl, axis=AX.X, op=ALU.max), [lg_a], [gmax_a])
        r_, ohg_a = row("ohg"); ohg = v3(r_, 4)
        dv(lambda: nc.vector.tensor_tensor(out=ohg, in0=gl, in1=bc(gmax.unsqueeze(2), 4), op=ALU.is_equal), [lg_a, gmax_a], [ohg_a])
        r_, gsh_a = row("gsh"); gsh = v3(r_, 4)
        dv(lambda: nc.vector.tensor_tensor(out=gsh, in0=gl, in1=bc(gmax.unsqueeze(2), 4), op=ALU.subtract), [lg_a, gmax_a], [gsh_a])
        r_, ge_a = row("ge"); ge = v3(r_, 4)
        S.op("act", lambda: nc.scalar.activation(out=ge, in_=gsh, func=AF.Exp), r=[gsh_a], w=[ge_a])
        r_, gsum_a = row("gsum"); gsum = r_[:, 0:4]
        dv(lambda: nc.vector.tensor_reduce(out=gsum, in_=ge, axis=AX.X, op=ALU.add), [ge_a], [gsum_a])
        r_, pen_a = row("pen"); pen = v3(r_, 4)
        dv(lambda: nc.vector.tensor_scalar(out=pen, in0=ohg, scalar1=1e9, scalar2=-1e9, op0=ALU.mult, op1=ALU.add), [ohg_a], [pen_a])
        r_, elm_a = row("elm"); elm = v3(r_, 32)
        for b in range(4):
            dv(lambda b=b: nc.vector.tensor_tensor(out=elm[:, b, :].rearrange("p (a b) -> p a b", b=8), in0=lg[:, b, 4:36].rearrange("p (a b) -> p a b", b=8),
                                                   in1=pen[:, b, :].unsqueeze(2).to_broadcast([128, 4, 8]), op=ALU.add), [lg_a, pen_a], [elm_a])
        r_, m8_a = row("m8"); m8 = v3(r_, 8)
        for b in range(4):
            dv(lambda b=b: nc.vector.max(out=m8[:, b, :], in_=elm[:, b, :]), [elm_a], [m8_a])
        top1 = m8[:, :, 0:1]
        top2 = m8[:, :, 1:2]
        r_, sel_a = row("sel"); sel = v3(r_, 32); sel_flat = r_[:, 0:128]
        dv(lambda: nc.vector.tensor_tensor(out=sel, in0=elm, in1=bc(top2, 32), op=ALU.is_ge), [elm_a, m8_a], [sel_a])
        r_, oh1_a = row("oh1"); oh1 = v3(r_, 32)
        dv(lambda: nc.vector.tensor_tensor(out=oh1, in0=elm, in1=bc(top1, 32), op=ALU.is_equal), [elm_a, m8_a], [oh1_a])
        r_, oh2_a = row("oh2"); oh2 = v3(r_, 32)
        dv(lambda: nc.vector.tensor_tensor(out=oh2, in0=sel, in1=oh1, op=ALU.subtract), [sel_a, oh1_a], [oh2_a])
        r_, esh_a = row("esh"); esh = v3(r_, 32)
        dv(lambda: nc.vector.tensor_tensor(out=esh, in0=elm, in1=bc(top1, 32), op=ALU.subtract), [elm_a, m8_a], [esh_a])
        dv(lambda: nc.vector.tensor_scalar(out=esh, in0=esh, scalar1=-80.0, scalar2=None, op0=ALU.max), [esh_a], [esh_a])
        r_, ee_a = row("ee"); ee = v3(r_, 32)
        S.op("act", lambda: nc.scalar.activation(out=ee, in_=esh, func=AF.Exp), r=[esh_a], w=[ee_a])
        r_, wraw_a = row("wraw"); wraw = v3(r_, 32)
        dv(lambda: nc.vector.tensor_tensor(out=wraw, in0=sel, in1=ee, op=ALU.mult), [sel_a, ee_a], [wraw_a])
        r_, ssum_a = row("ssum"); ssum = r_[:, 0:4]
        dv(lambda: nc.vector.tensor_reduce(out=ssum, in_=wraw, axis=AX.X, op=ALU.add), [wraw_a], [ssum_a])
        dv(lambda: nc.vector.tensor_tensor(out=ssum, in0=ssum, in1=gsum, op=ALU.mult), [ssum_a, gsum_a], [ssum_a])
        r_, coef_a = row("coef"); coef = r_[:, 0:4]
        dv(lambda: nc.vector.reciprocal(out=coef, in_=ssum), [ssum_a], [coef_a])
        r_, wmat_a = row("wmat"); wmat = v3(r_, 32)
        dv(lambda: nc.vector.tensor_tensor(out=wmat, in0=wraw, in1=bc(coef.unsqueeze(2), 32), op=ALU.mult), [wraw_a, coef_a], [wmat_a])
        r_, w12_a = row("w12"); w12 = r_[:, 0:8].rearrange("p (a b) -> p a b", b=4)
        r_, t1_a = row("t1"); t1 = v3(r_, 32)
        dv(lambda: nc.vector.tensor_tensor(out=t1, in0=wmat, in1=oh1, op=ALU.mult), [wmat_a, oh1_a], [t1_a])
        dv(lambda: nc.vector.tensor_reduce(out=w12[:, 0, :], in_=t1, axis=AX.X, op=ALU.add), [t1_a], [w12_a])
        r_, t2_a = row("t2"); t2 = v3(r_, 32)
        dv(lambda: nc.vector.tensor_tensor(out=t2, in0=wmat, in1=oh2, op=ALU.mult), [wmat_a, oh2_a], [t2_a])
        dv(lambda: nc.vector.tensor_reduce(out=w12[:, 1, :], in_=t2, axis=AX.X, op=ALU.add), [t2_a], [w12_a])
        pr = psum()
        mm_group(pr.f[:, 0:128], [(triU, sel_flat)], pr.acc, [cst.a(), sel_a])
        mm_group(pr.f[:, 128:256], [(ones_f, sel_flat)], pr.acc, [cst.a(), sel_a])
        r_, dest_a = row("dest"); dest = v3(r_, 32)
        for b in range(4):
            dv(lambda b=b: nc.vector.tensor_tensor(out=dest[:, b, :], in0=pr.f[:, 32 * b:32 * b + 32], in1=carry.ap, op=ALU.add), [pr.acc, carry.a()], [dest_a])
            dv(lambda b=b: nc.vector.tensor_tensor(out=carry.ap, in0=pr.f[:, 128 + 32 * b:128 + 32 * b + 32], in1=carry.ap, op=ALU.add), [pr.acc, carry.a()], [carry.a()])
        r_, ovf_a = row("ovf"); ovf = v3(r_, 32)
        dv(lambda: nc.vector.tensor_scalar(out=ovf, in0=dest, scalar1=float(CAP), scalar2=None, op0=ALU.is_ge), [dest_a], [ovf_a])
        dv(lambda: nc.vector.tensor_tensor(out=dest, in0=dest, in1=ebase.unsqueeze(1).to_broadcast([128, 4, 32]), op=ALU.add), [dest_a, cst.a()], [dest_a])
        r_, tr_a = row("tr"); tr = v3(r_, 32)
        dv(lambda: nc.vector.tensor_scalar(out=tr, in0=dest, scalar1=-1.0, scalar2=trashcol, op0=ALU.mult, op1=ALU.add), [dest_a, cst.a()], [tr_a])
        dv(lambda: nc.vector.tensor_tensor(out=tr, in0=tr, in1=ovf, op=ALU.mult), [tr_a, ovf_a], [tr_a])
        dv(lambda: nc.vector.tensor_tensor(out=dest, in0=dest, in1=tr, op=ALU.add), [dest_a, tr_a], [dest_a])
        r_, d12_a = row("d12"); d12 = r_[:, 0:8].rearrange("p (a b) -> p a b", b=4)
        r_, t3_a = row("t3"); t3 = v3(r_, 32)
        dv(lambda: nc.vector.tensor_tensor(out=t3, in0=dest, in1=oh1, op=ALU.mult), [dest_a, oh1_a], [t3_a])
        dv(lambda: nc.vector.tensor_reduce(out=d12[:, 0, :], in_=t3, axis=AX.X, op=ALU.add), [t3_a], [d12_a])
        r_, t4_a = row("t4"); t4 = v3(r_, 32)
        dv(lambda: nc.vector.tensor_tensor(out=t4, in0=dest, in1=oh2, op=ALU.mult), [dest_a, oh2_a], [t4_a])
        dv(lambda: nc.vector.tensor_reduce(out=d12[:, 1, :], in_=t4, axis=AX.X, op=ALU.add), [t4_a], [d12_a])
        r_, okm_a = row("okm"); okm = r_[:, 0:8].rearrange("p (a b) -> p a b", b=4)
        dv(lambda: nc.vector.tensor_scalar(out=okm, in0=d12, scalar1=float(NE * CAP) - 0.5, scalar2=None, op0=ALU.is_lt), [d12_a], [okm_a])
        dv(lambda: nc.vector.tensor_tensor(out=w12, in0=w12, in1=okm, op=ALU.mult), [w12_a, okm_a], [w12_a])
        for kk in range(2):
            dv(lambda kk=kk: nc.vector.tensor_copy(out=wts_all.ap[:, blk0:blk0 + 4, kk], in_=w12[:, kk, :]), [w12_a], [wts_all.a(blk0, blk0 + 4)])
            dv(lambda kk=kk: nc.vector.tensor_copy(out=idx_all.ap[:, blk0:blk0 + 4, kk], in_=d12[:, kk, :]), [d12_a], [idx_all.a(blk0, blk0 + 4)])

    TILES = [(seg, st, SEGS[seg][1] // NT) for seg in range(2) for st in range(SEGS[seg][1] // NT)]
    for ti in range(len(TILES)):
        supertile(ti)

    A.off = mark_phase
    ln_g2 = A.buf("ln_g2", [D], F32)
    ln_b2 = A.buf("ln_b2", [D], F32)
    ld("sp", ln_g2.ap, ln_d[4], [ln_g2.a()])
    ld("sp", ln_b2.ap, ln_d[5], [ln_b2.a()])
    mark_p2 = A.off
    wgu = [A.buf(f"wgu{i}", [8, D], BF16) for i in range(3)]
    wdn = [A.buf(f"wdn{i}", [4, D], BF16) for i in range(3)]
    xg = [A.buf(f"xg{i}", [4, D], BF16) for i in range(2)]
    XT = A.buf("XT", [8, 512], BF16)
    sgb = [A.buf(f"sg{i}", [512], F32) for i in range(2)]
    hT = A.buf("hT", [4, 512], BF16)
    yst = [A.buf(f"yst{i}", [4, D], F32) for i in range(2)]

    def load_expert_w(e):
        wg, wd_ = wgu[e % 3], wdn[e % 3]
        ld("pool", wg.ap, w_gu_d[e].rearrange("(c p) n -> p c n", p=128), [wg.a()])
        ld("pool", wd_.ap, w_down_d[e].rearrange("(c p) n -> p c n", p=128), [wd_.a()])

    def load_xg(e):
        xg_ = xg[e % 2]
        S.op("sp", lambda: nc.sync.dma_start(out=xg_.ap, in_=xs_scr[e * CAP:(e + 1) * CAP, :].rearrange("(b p) d -> p b d", p=128)),
             r=[("xs_scr", 0, NE * CAP)], w=[xg_.a()], dma=True)

    load_expert_w(0)
    load_expert_w(1)
    load_xg(0)
    for e in range(NE):
        wg, wd_, xg_, ys_ = wgu[e % 3], wdn[e % 3], xg[e % 2], yst[e % 2]
        if e + 2 < NE:
            load_expert_w(e + 2)
        if e + 1 < NE:
            load_xg(e + 1)
        for i in range(4):
            ps = psum()
            for kk in range(2):
                k = 2 * i + kk
                for b in range(4):
                    S.op("pe", lambda k=k, kk=kk, b=b, ps=ps, xg_=xg_: nc.tensor.transpose(ps.b[:, kk * 512 + b * 128: kk * 512 + (b + 1) * 128],
                                                                                           xg_.ap[:, b, k * 128:(k + 1) * 128], ident_b.ap),
                         r=[xg_.a(), ident_b.a()], w=[ps.acc])
            eng = "act" if i % 2 == 0 else "dve"
            if eng == "act":
                S.op("act", lambda ps=ps, i=i: nc.scalar.copy(out=XT.ap[:, 2 * i:2 * i + 2, :], in_=ps.b[:, :].rearrange("p (a b) -> p a b", b=512)),
                     r=[ps.acc], w=[XT.a(2 * i, 2 * i + 2)])
            else:
                S.op("dve", lambda ps=ps, i=i: nc.vector.tensor_copy(out=XT.ap[:, 2 * i:2 * i + 2, :], in_=ps.b[:, :].rearrange("p (a b) -> p a b", b=512)),
                     r=[ps.acc], w=[XT.a(2 * i, 2 * i + 2)])
        for m in range(4):
            pg = psum()
            mm_group(pg.f[:, :], [(wg.ap[:, k, m * 128:(m + 1) * 128], XT.ap[:, k, :]) for k in range(8)], pg.acc, [wg.a(), XT.a()])
            pu = psum()
            mm_group(pu.f[:, :], [(wg.ap[:, k, 512 + m * 128:512 + (m + 1) * 128], XT.ap[:, k, :]) for k in range(8)], pu.acc, [wg.a(), XT.a()])
            sg = sgb[m % 2]
            S.op("act", lambda pg=pg, sg=sg: nc.scalar.activation(out=sg.ap, in_=pg.f[:, :], func=AF.Silu), r=[pg.acc], w=[sg.a()])
            S.op("dve", lambda pu=pu, sg=sg, m=m: nc.vector.tensor_tensor(out=hT.ap[:, m, :], in0=sg.ap, in1=pu.f[:, :], op=ALU.mult),
                 r=[pu.acc, sg.a()], w=[hT.a(m)])
        for b in range(4):
            for n in range(2):
                ps = psum()
                mm_group(ps.f[:, :], [(hT.ap[:, j, b * 128:(b + 1) * 128], wd_.ap[:, j, n * 512:(n + 1) * 512]) for j in range(4)], ps.acc, [hT.a(), wd_.a()])
                if n == 0:
                    S.op("act", lambda ps=ps, b=b, n=n, ys_=ys_: nc.scalar.copy(out=ys_.ap[:, b, n * 512:(n + 1) * 512], in_=ps.f[:, :]), r=[ps.acc], w=[ys_.a(b)])
                else:
                    S.op("dve", lambda ps=ps, b=b, n=n, ys_=ys_: nc.vector.tensor_copy(out=ys_.ap[:, b, n * 512:(n + 1) * 512], in_=ps.f[:, :]), r=[ps.acc], w=[ys_.a(b)])
        S.op("sp", lambda e=e, ys_=ys_: nc.sync.dma_start(out=ys_scr[e * CAP:(e + 1) * CAP, :].rearrange("(b p) d -> p b d", p=128), in_=ys_.ap),
             r=[ys_.a()], w=[("ys_scr", e, e + 1)], dma=True)

    A.off = mark_p2
    NB3 = 4
    ya = [A.buf(f"ya{i}", [D], F32) for i in range(NB3)]
    yb = [A.buf(f"yb{i}", [D], F32) for i in range(NB3)]
    x1r = [A.buf(f"x1r{i}", [D], F32) for i in range(NB3)]
    z2 = [A.buf(f"z2{i}", [D], F32) for i in range(4)]
    outb = [A.buf(f"outb{i}", [D], F32) for i in range(3)]
    lnstat2 = [A.buf(f"lnstat2{i}", [16], F32) for i in range(4)]

    def p3_loads(blk):
        i = blk % NB3
        for kk, dst in ((0, ya[i]), (1, yb[i])):
            S.op("pool", lambda dst=dst, kk=kk: nc.gpsimd.indirect_dma_start(
                out=dst.ap, out_offset=None, in_=ys_scr[:, :],
                in_offset=bass.IndirectOffsetOnAxis(ap=idx_all.ap[:, blk, kk:kk + 1].bitcast(U32), axis=0)),
                r=[("ys_scr", 0, NE + 1), idx_all.a(blk)], w=[dst.a()], dma=True)
        S.op("act", lambda: nc.scalar.dma_start(out=x1r[i].ap, in_=x1_scr[blk * 128:(blk + 1) * 128, :]),
             r=[("x1_scr", blk, blk + 1)], w=[x1r[i].a()], dma=True)

    for bb in ya + yb:
        S.op("pool", lambda bb=bb: nc.gpsimd.memset(bb.ap, 0.0), w=[bb.a()])
    PD = NB3 - 1
    for b_ in range(PD):
        p3_loads(b_)

    def p3_finish_a(blk):
        j = blk % 4
        st_ = lnstat2[j]
        S.op("dve", lambda: nc.vector.tensor_scalar(out=st_.ap[:, 11:12], in0=st_.ap[:, 12:13], scalar1=st_.ap[:, 15:16], scalar2=-1.0, op0=ALU.mult, op1=ALU.mult),
             r=[st_.a(0)], w=[st_.a(0)])
        S.op("act", lambda: nc.scalar.activation(out=z2[j].ap, in_=z2[j].ap, func=AF.Identity, bias=st_.ap[:, 11:12], scale=st_.ap[:, 15:16]),
             r=[z2[j].a(), st_.a(0)], w=[z2[j].a()])

    def p3_finish_b(blk):
        j = blk % 4
        k = blk % 3
        S.op("dve", lambda: nc.vector.tensor_tensor(out=z2[j].ap, in0=z2[j].ap, in1=ln_g2.ap, op=ALU.mult), r=[z2[j].a(), ln_g2.a()], w=[z2[j].a()])
        S.op("dve", lambda: nc.vector.tensor_tensor(out=outb[k].ap, in0=z2[j].ap, in1=ln_b2.ap, op=ALU.add), r=[z2[j].a(), ln_b2.a()], w=[outb[k].a()])
        S.op("sp", lambda: nc.sync.dma_start(out=y_out[blk * 128:(blk + 1) * 128, :], in_=outb[k].ap),
             r=[outb[k].a()], w=[("y_out", blk, blk + 1)], dma=True)

    for blk in range(NBLK):
        if blk + PD < NBLK:
            p3_loads(blk + PD)
        i = blk % NB3
        j = blk % 4
        S.op("act", lambda i=i: nc.scalar.mul(out=x1r[i].ap, in_=x1r[i].ap, mul=ALPHA), r=[x1r[i].a()], w=[x1r[i].a()])
        S.op("dve", lambda i=i, j=j, blk=blk: nc.vector.scalar_tensor_tensor(out=z2[j].ap, in0=ya[i].ap, scalar=wts_all.ap[:, blk, 0:1], in1=x1r[i].ap, op0=ALU.mult, op1=ALU.add),
             r=[ya[i].a(), x1r[i].a(), wts_all.a(blk)], w=[z2[j].a()])
        S.op("dve", lambda i=i, j=j, blk=blk: nc.vector.scalar_tensor_tensor(out=z2[j].ap, in0=yb[i].ap, scalar=wts_all.ap[:, blk, 1:2], in1=z2[j].ap, op0=ALU.mult, op1=ALU.add),
             r=[yb[i].a(), z2[j].a(), wts_all.a(blk)], w=[z2[j].a()])
        ln_stats(z2[j].ap, z2[j].a(), lnstat2[j])
        if blk >= 1:
            p3_finish_a(blk - 1)
        if blk >= 2:
            p3_finish_b(blk - 2)
    p3_finish_a(NBLK - 1)
    p3_finish_b(NBLK - 2)
    p3_finish_b(NBLK - 1)

    sems_c = {e: nc.alloc_semaphore(f"s_{e}") for e in Sched.COMPUTE}
    sems_d = {"sp": [nc.alloc_semaphore(f"d_sp{i}") for i in range(24)],
              "pool": [nc.alloc_semaphore(f"d_pool{i}") for i in range(24)],
              "act": [nc.alloc_semaphore(f"d_act{i}") for i in range(8)]}
    nw = S.emit(sems_c, sems_d)
    print(f"[kernel] ops={len(S.ops)} waits={nw}")
    return nc


def _t5_bucket_np(rel):
    nb = 16
    max_exact = 8
    ret = np.where(rel > 0, nb, 0)
    n = np.abs(rel)
    nf = np.maximum(n, 1).astype(np.float32)
    large = max_exact + (np.log(nf / max_exact) / math.log(128 / max_exact) * (nb - max_exact)).astype(np.int32)
    large = np.minimum(large, nb - 1)
    return ret + np.where(n < max_exact, n, large)


_NC_CACHE = {}


def kernel(x_prompt, x_sample, mem_prompt, mem_sample, ln_in_g, ln_in_b, rel_bias_table, w_in, b_in,
           w_pool, pool_scale, p_pool, sink, p_attn, w_mem_kv, p_mem, w_out, ln1_g, ln1_b,
           w_router_group, w_router_expert, w_gu, w_down, ln2_g, ln2_b):
    f32 = np.float32
    A_ = lambda a: np.ascontiguousarray(np.asarray(a, dtype=f32))
    x_prompt, x_sample, mem_prompt, mem_sample = map(A_, (x_prompt, x_sample, mem_prompt, mem_sample))
    w_in0 = A_(w_in)[0]
    b_in0 = A_(b_in)[0]
    o1, o2, o3, o4, o5 = 512, 1536, 1792, 2048, 2560

    qcols = []
    for gg in range(2):
        for j in range(4):
            for g_ in (2 * gg, 2 * gg + 1):
                h = 4 * g_ + j
                qcols.extend(range(o1 + 64 * h, o1 + 64 * h + 64))
    res_cols = list(range(0, 512)) + qcols + list(range(o2, o3)) + list(range(o3, o4)) + list(range(o4, o5))
    res_cols = np.array(res_cols)
    kmaj = lambda w: np.ascontiguousarray(w.reshape(w.shape[0] // 128, 128, w.shape[1]).transpose(1, 0, 2))
    w_res = kmaj(w_in0[:, res_cols])
    b_res_full = b_in0[res_cols]
    bcols = np.concatenate([b_res_full[0:512], b_res_full[512:1536], b_res_full[1536:1792], b_res_full[2048:2560]])
    b_res = np.zeros((128, 20), f32)
    b_res[:, 0:14] = bcols[0:1792].reshape(14, 128).T
    b_res[:, 16:20] = bcols[1792:2304].reshape(4, 128).T
    bv_bc = np.ascontiguousarray(np.broadcast_to(b_res_full[1792:2048][None, :], (128, 256)))
    b_gate = np.ascontiguousarray(b_in0[o5:].reshape(24, 128).T)
    wg_k = kmaj(w_in0[:, o5:])
    pp_k = kmaj(A_(p_pool)[0])
    pa_k = kmaj(A_(p_attn)[0])
    pm_k = kmaj(A_(p_mem)[0])
    wst = np.empty((8, 128, 40, 128), f32)
    for c in range(8):
        for r in range(3):
            wst[c, :, r * 8:(r + 1) * 8, :] = wg_k[:, :, r * 1024 + c * 128: r * 1024 + (c + 1) * 128]
        wst[c, :, 24:28, :] = pp_k[:, :, c * 128:(c + 1) * 128]
        wst[c, :, 28:36, :] = pa_k[:, :, c * 128:(c + 1) * 128]
        wst[c, :, 36:40, :] = pm_k[:, :, c * 128:(c + 1) * 128]
    wst = wst.reshape(8, 128, 5120)
    w_out_l = kmaj(A_(w_out)[0])
    w_pool_l = np.ascontiguousarray(A_(w_pool)[0].transpose(1, 0, 2))
    pscale = np.ascontiguousarray(A_(pool_scale)[0].reshape(4, 128).T)
    w_memkv_l = kmaj(A_(w_mem_kv)[0])
    bc = lambda v: np.broadcast_to(np.asarray(v, f32).reshape(1, -1), (128, np.asarray(v).size))
    ln_bc = np.ascontiguousarray(np.stack([bc(ln_in_g), bc(ln_in_b), bc(A_(ln1_g)[0]), bc(A_(ln1_b)[0]), bc(A_(ln2_g)[0]), bc(A_(ln2_b)[0])]))
    kk = np.arange(128)[:, None, None]
    rr = np.arange(3)[None, :, None]
    qq = np.arange(128)[None, None, :]
    rel = (rr - 1) * 128 + kk - qq
    bidx = _t5_bucket_np(rel)
    tab = A_(rel_bias_table)
    biasT = np.ascontiguousarray(tab[bidx].transpose(0, 1, 3, 2))
    sink_bc = np.ascontiguousarray(bc(A_(sink)[0]))
    wr_l = kmaj(np.concatenate([A_(w_router_group)[0], A_(w_router_expert)[0]], axis=1))
    consts = np.zeros((128, 419), f32)
    consts[:, 0:128] = np.eye(128, dtype=f32)
    consts[:, 128:256] = np.triu(np.ones((128, 128), f32), k=1)
    consts[:, 256:384] = 1.0
    consts[:, 384:416] = (np.arange(32) * CAP)[None, :]
    consts[:, 416] = -0.5
    consts[:, 417] = LN_EPS
    consts[:, 418] = NE * CAP + np.arange(128)
    w_gu0 = A_(w_gu)[0]
    w_down0 = A_(w_down)[0]

    in_maps = []
    for c in range(NCORES):
        b, half = c // 2, c % 2
        xh = np.zeros((XH_ROWS, D), f32)
        flags = np.ones((128, 4), f32)
        invc = np.empty((128, 2, 2, 4, 16), f32)
        for s, (xsrc, ntok) in enumerate(((x_prompt, P_TOK), (x_sample, S_TOK))):
            Sfull = 2 * ntok
            t0 = half * ntok
            lo, hi = t0 - 128, t0 + ntok + 128
            clo, chi = max(lo, 0), min(hi, Sfull)
            base = SEGS[s][0]
            xh[base + (clo - lo): base + (chi - lo)] = xsrc[b, clo:chi]
            flags[:, 2 * s] = 1.0 if lo >= 0 else 0.0
            flags[:, 2 * s + 1] = 1.0 if hi <= Sfull else 0.0
            for g_ in range(4):
                w_ = 2 << g_
                for e_, tpos in ((0, t0 + np.arange(16)), (1, t0 + ntok - 16 + np.arange(16))):
                    cnt = np.minimum(tpos + w_ // 2, Sfull) - np.maximum(tpos - w_ // 2, 0)
                    invc[:, s, e_, g_, :] = (1.0 / cnt.astype(np.float64)).astype(f32)[None, :]
        in_maps.append({
            "xh": xh, "mem": np.ascontiguousarray(np.stack([mem_prompt[b], mem_sample[b]])), "flags": flags, "invcnt": invc,
            "w_res": w_res, "b_res": b_res, "bv_bc": bv_bc, "b_gate": b_gate, "wst": wst, "w_out_l": w_out_l,
            "w_pool_l": w_pool_l, "pscale": pscale, "w_memkv_l": w_memkv_l, "ln_bc": ln_bc, "biasT": biasT,
            "sink_bc": sink_bc, "wr_l": wr_l, "w_gu": w_gu0, "w_down": w_down0, "consts": consts,
        })
    if "nc" not in _NC_CACHE:
        _NC_CACHE["nc"] = build_program()
    nc = _NC_CACHE["nc"]
    res = run_bass_kernel_spmd(nc, in_maps, core_ids=list(range(NCORES)))
    y_p = np.empty((4, 8192, D), f32)
    y_s = np.empty((4, 4096, D), f32)
    for c in range(NCORES):
        b, half = c // 2, c % 2
        y = res.results[c]["y"]
        y_p[b, half * P_TOK:(half + 1) * P_TOK] = y[:P_TOK]
        y_s[b, half * S_TOK:(half + 1) * S_TOK] = y[P_TOK:]
    return (y_p, y_s)
```
